# Optimizing a Trainium2 kernel written in Bass

```python
import jax, jax.numpy as jnp
from jax import lax
import numpy as np

D_MODEL = 1024
BATCH = 4
SEQ = 8192
DEPTH = 4

CHUNK = 128
FOURIER_W = D_MODEL // 4
N_FGROUPS = 4
ML_W = 3 * D_MODEL // 8
RET_W = D_MODEL - FOURIER_W - ML_W
D_MIX = FOURIER_W + ML_W + RET_W
ML_HEADS = 4
ML_DV = ML_W // ML_HEADS
ML_DK = ML_DV // 2
RET_HEADS = 4
RET_DV = RET_W // RET_HEADS
RET_DK = RET_DV // 2
CONV_W = 3
ROPE_BASE = 10000.0
RET_GAMMA_EXP0 = 5.0
RET_BWD_EXP_OFFSET = 0.5
N_EXPERTS = 32
TOP_K = 4
D_FF = D_MODEL
SWIGLU_ALPHA = 1.702
SWIGLU_LIMIT = 7.0
MOE_BLOCK = 128
DEEPNORM_ALPHA = (2.0 * DEPTH) ** 0.25
DEEPNORM_BETA = (8.0 * DEPTH) ** -0.25
LN_EPS = 1e-5

COL_F = 0
COL_MQK = COL_F + FOURIER_W
COL_MV = COL_MQK + 2 * ML_HEADS * ML_DK
COL_MO = COL_MV + ML_W
COL_MG = COL_MO + ML_W
COL_RQ = COL_MG + 4 * ML_HEADS
COL_RK = COL_RQ + RET_HEADS * RET_DK
COL_RV = COL_RK + RET_HEADS * RET_DK
COL_RG = COL_RV + RET_W
PROJ_W = COL_RG + RET_W

kernel_name = "hybrid_fnet_mlstm_retention_moe_deepnorm"


def _layernorm(x, g, b):
    xf = x.astype(jnp.float32)
    mu = xf.mean(-1, keepdims=True)
    var = jnp.square(xf - mu).mean(-1, keepdims=True)
    return ((xf - mu) * lax.rsqrt(var + LN_EPS) * g + b).astype(x.dtype)


def _head_norm(h, w):
    mu = h.mean(-1, keepdims=True)
    var = jnp.square(h - mu).mean(-1, keepdims=True)
    y = (h - mu) * lax.rsqrt(var + LN_EPS)
    return y.reshape(h.shape[0], h.shape[1], -1) * w


def _to_chunks(t):
    Bb, S, H, d = t.shape
    return t.reshape(Bb, S // CHUNK, CHUNK, H, d).transpose(0, 3, 1, 2, 4)


def _gate_chunks(t):
    Bb, S, H = t.shape
    return t.reshape(Bb, S // CHUNK, CHUNK, H).transpose(0, 3, 1, 2)


def _from_chunks(t):
    Bb, H, N, L, d = t.shape
    return t.transpose(0, 2, 3, 1, 4).reshape(Bb, N * L, H, d)


def _centred_dwconv(u, w):
    pad = CONV_W // 2
    S = u.shape[1]
    up = jnp.pad(u, ((0, 0), (pad, pad), (0, 0)))
    return sum(up[:, j:j + S] * w[j] for j in range(CONV_W))


def _rotary(t):
    S, d = t.shape[1], t.shape[3]
    inv = 1.0 / (ROPE_BASE ** (jnp.arange(0, d, 2, dtype=jnp.float32) / d))
    ang = jnp.arange(S, dtype=jnp.float32)[:, None] * inv[None, :]
    cos = jnp.cos(ang)[None, :, None, :]
    sin = jnp.sin(ang)[None, :, None, :]
    t1, t2 = t[..., 0::2], t[..., 1::2]
    return jnp.stack([t1 * cos - t2 * sin, t1 * sin + t2 * cos], axis=-1).reshape(t.shape)


def _fourier_mixer(u):
    Bb, S, _ = u.shape
    ug = u.astype(jnp.float32).reshape(Bb, S, N_FGROUPS, FOURIER_W // N_FGROUPS)
    y = jnp.fft.fft2(ug, axes=(1, 3), norm="ortho").real
    return y.reshape(Bb, S, FOURIER_W)


def _mlstm_chunkwise(q, k, v, i_pre, f_pre):
    qc, kc, vc = _to_chunks(q), _to_chunks(k), _to_chunks(v)
    ic = _gate_chunks(i_pre)
    a = jnp.cumsum(jax.nn.log_sigmoid(_gate_chunks(f_pre)), axis=-1)
    a_end = a[..., -1]
    w_key = a_end[..., None] - a + ic
    m_chunk = w_key.max(-1)
    p = jnp.exp(w_key - m_chunk[..., None])
    c_chunk = jnp.einsum('bhnsv,bhnsk->bhnvk', vc * p[..., None], kc)
    n_chunk = jnp.einsum('bhnsk,bhns->bhnk', kc, p)

    def step(carry, xs):
        c, n, m = carry
        ae, mc, cc, nc = xs
        m_new = jnp.maximum(ae + m, mc)
        s_prev = jnp.exp(ae + m - m_new)
        s_cur = jnp.exp(mc - m_new)
        c_new = s_prev[..., None, None] * c + s_cur[..., None, None] * cc
        n_new = s_prev[..., None] * n + s_cur[..., None] * nc
        return (c_new, n_new, m_new), (c, n, m)

    Bb, H = qc.shape[:2]
    dk, dv = qc.shape[-1], vc.shape[-1]
    init = (jnp.zeros((Bb, H, dv, dk), jnp.float32), jnp.zeros((Bb, H, dk), jnp.float32),
            jnp.zeros((Bb, H), jnp.float32))
    xs = tuple(jnp.moveaxis(t, 2, 0) for t in (a_end, m_chunk, c_chunk, n_chunk))
    _, (c_prev, n_prev, m_prev) = lax.scan(step, init, xs)
    c_prev = jnp.moveaxis(c_prev, 0, 2)
    n_prev = jnp.moveaxis(n_prev, 0, 2)
    m_prev = jnp.moveaxis(m_prev, 0, 2)

    inter_log = a + m_prev[..., None]
    causal = jnp.tril(jnp.ones((CHUNK, CHUNK), dtype=bool))
    d_log = jnp.where(causal, a[..., :, None] - a[..., None, :] + ic[..., None, :], -jnp.inf)
    m_t = jnp.maximum(inter_log, d_log.max(-1))
    scale_inter = jnp.exp(inter_log - m_t)
    scores = jnp.einsum('bhntk,bhnsk->bhnts', qc, kc) * jnp.exp(d_log - m_t[..., None])
    num = (jnp.einsum('bhnts,bhnsv->bhntv', scores, vc)
           + scale_inter[..., None] * jnp.einsum('bhnvk,bhntk->bhntv', c_prev, qc))
    den = scores.sum(-1) + scale_inter * jnp.einsum('bhnk,bhntk->bhnt', n_prev, qc)
    h = num / jnp.maximum(jnp.abs(den), jnp.exp(-m_t))[..., None]
    return _from_chunks(h)


def _mlstm_mixer(qk, v, o, gates, norm_w):
    Bb, S, _ = v.shape
    q, k = jnp.split(qk, 2, axis=-1)
    q = q.reshape(Bb, S, ML_HEADS, ML_DK)
    k = k.reshape(Bb, S, ML_HEADS, ML_DK) * ML_DK ** -0.5
    v = v.reshape(Bb, S, ML_HEADS, ML_DV)
    i_f, f_f, i_b, f_b = jnp.split(gates, 4, axis=-1)
    fl = lambda t: jnp.flip(t, axis=1)
    h_fwd = _mlstm_chunkwise(q, k, v, i_f, f_f)
    h_bwd = fl(_mlstm_chunkwise(fl(q), fl(k), fl(v), fl(i_b), fl(f_b)))
    h = jax.nn.sigmoid(o).reshape(Bb, S, ML_HEADS, ML_DV) * (h_fwd + h_bwd)
    return _head_norm(h, norm_w)


def _ret_log_gamma(offset):
    return jnp.log1p(-jnp.exp2(-(RET_GAMMA_EXP0 + offset) - jnp.arange(RET_HEADS, dtype=jnp.float32)))


def _retention_chunkwise(q, k, v, log_gamma, inclusive):
    qc, kc, vc = _to_chunks(q), _to_chunks(k), _to_chunks(v)
    idx = jnp.arange(CHUNK, dtype=jnp.float32)
    lg = log_gamma[:, None]
    diff = idx[:, None] - idx[None, :]
    mask = (diff >= 0) if inclusive else (diff > 0)
    decay = jnp.where(mask[None], jnp.exp(log_gamma[:, None, None] * jnp.maximum(diff, 0.0)[None]), 0.0)
    w_key = jnp.exp(lg * (CHUNK - 1 - idx))
    w_inter = jnp.exp(lg * (idx + 1.0))
    g_chunk = jnp.exp(log_gamma * CHUNK)
    s_chunk = jnp.einsum('bhnsv,bhnsk,hs->bhnvk', vc, kc, w_key)

    def step(state, s_c):
        return g_chunk[None, :, None, None] * state + s_c, state

    Bb, H = qc.shape[:2]
    init = jnp.zeros((Bb, H, vc.shape[-1], kc.shape[-1]), jnp.float32)
    _, s_prev = lax.scan(step, init, jnp.moveaxis(s_chunk, 2, 0))
    s_prev = jnp.moveaxis(s_prev, 0, 2)
    scores = jnp.einsum('bhntk,bhnsk->bhnts', qc, kc) * decay[None, :, None]
    y = (jnp.einsum('bhnts,bhnsv->bhntv', scores, vc)
         + w_inter[None, :, None, :, None] * jnp.einsum('bhnvk,bhntk->bhntv', s_prev, qc))
    return _from_chunks(y)


def _retention_mixer(q, k, v, g, norm_w):
    Bb, S, _ = q.shape
    q = _rotary(q.reshape(Bb, S, RET_HEADS, RET_DK))
    k = _rotary(k.reshape(Bb, S, RET_HEADS, RET_DK)) * RET_DK ** -0.5
    v = v.reshape(Bb, S, RET_HEADS, RET_DV)
    fl = lambda t: jnp.flip(t, axis=1)
    y_fwd = _retention_chunkwise(q, k, v, _ret_log_gamma(0.0), True)
    y_bwd = fl(_retention_chunkwise(fl(q), fl(k), fl(v), _ret_log_gamma(RET_BWD_EXP_OFFSET), False))
    return jax.nn.silu(g) * _head_norm(y_fwd + y_bwd, norm_w)


def _moe(h, w_router, b_router, w1, b1, w2, b2):
    Bb, S, D = h.shape
    T = Bb * S
    xt = h.reshape(T, D)
    logits = (xt @ w_router + b_router).astype(jnp.float32)
    top_v, top_i = lax.top_k(logits, TOP_K)
    gates = jax.nn.softmax(top_v, axis=-1)
    N = T * TOP_K
    e_flat = top_i.reshape(N)
    tok_flat = jnp.arange(N, dtype=jnp.int32) // TOP_K
    order = jnp.argsort(e_flat)
    e_sorted, tok_sorted = e_flat[order], tok_flat[order]
    g_sorted = gates.reshape(N)[order]
    sizes = jnp.bincount(e_flat, length=N_EXPERTS)
    starts = jnp.cumsum(sizes) - sizes
    psizes = (sizes + MOE_BLOCK - 1) // MOE_BLOCK * MOE_BLOCK
    pends = jnp.cumsum(psizes)
    pstarts = pends - psizes
    pos = pstarts[e_sorted] + jnp.arange(N, dtype=jnp.int32) - starts[e_sorted]
    P = N + N_EXPERTS * MOE_BLOCK
    NB = P // MOE_BLOCK
    src_tok = jnp.zeros((P,), jnp.int32).at[pos].set(tok_sorted)
    gate_pad = jnp.zeros((P,), jnp.float32).at[pos].set(g_sorted)
    block_e = jnp.minimum(jnp.searchsorted(pends, jnp.arange(NB, dtype=jnp.int32) * MOE_BLOCK, side='right'),
                          N_EXPERTS - 1)
    x_pad = xt[src_tok].reshape(NB, MOE_BLOCK, D)

    def expert_block(args):
        xb, e = args
        hc = xb @ w1[e] + b1[e]
        gate, up = hc[:, :D_FF], hc[:, D_FF:]
        gate = jnp.minimum(gate, SWIGLU_LIMIT)
        up = jnp.clip(up, -SWIGLU_LIMIT, SWIGLU_LIMIT)
        glu = gate * jax.nn.sigmoid(SWIGLU_ALPHA * gate)
        return ((up + 1.0) * glu) @ w2[e] + b2[e]

    y_pad = lax.map(expert_block, (x_pad, block_e)).reshape(P, D)
    y = jnp.zeros((T, D), h.dtype).at[src_tok].add(y_pad * gate_pad[:, None].astype(h.dtype))
    return y.reshape(Bb, S, D)


def setup_inputs(seed: int = 0) -> dict:
    key = jax.random.key(seed)
    ks = jax.random.split(key, 20)
    f32 = jnp.float32

    def nrm(k, shape, scale):
        return jax.random.normal(k, shape, f32) * scale

    x = nrm(ks[0], (BATCH, SEQ, D_MODEL), 1.0)
    emb_ln_g = 1.0 + nrm(ks[1], (D_MODEL,), 0.01)
    emb_ln_b = nrm(ks[2], (D_MODEL,), 0.01)
    w_in = nrm(ks[3], (DEPTH, D_MODEL, PROJ_W), D_MODEL ** -0.5)
    b_in = nrm(ks[4], (DEPTH, PROJ_W), 0.01)
    f_bias = jnp.linspace(3.0, 6.0, ML_HEADS, dtype=f32)
    b_in = b_in.at[:, COL_MG + ML_HEADS:COL_MG + 2 * ML_HEADS].add(f_bias)
    b_in = b_in.at[:, COL_MG + 3 * ML_HEADS:COL_MG + 4 * ML_HEADS].add(f_bias)
    conv_w = nrm(ks[5], (DEPTH, CONV_W, 2 * ML_HEADS * ML_DK), CONV_W ** -0.5)
    ml_norm_w = 1.0 + nrm(ks[6], (DEPTH, ML_W), 0.01)
    ret_norm_w = 1.0 + nrm(ks[7], (DEPTH, RET_W), 0.01)
    w_out = nrm(ks[8], (DEPTH, D_MIX, D_MODEL), DEEPNORM_BETA * D_MIX ** -0.5)
    ln1_g = 1.0 + nrm(ks[9], (DEPTH, D_MODEL), 0.01)
    ln1_b = nrm(ks[10], (DEPTH, D_MODEL), 0.01)
    w_router = nrm(ks[11], (DEPTH, D_MODEL, N_EXPERTS), D_MODEL ** -0.5)
    b_router = nrm(ks[12], (DEPTH, N_EXPERTS), 0.01)
    w1 = nrm(ks[13], (DEPTH, N_EXPERTS, D_MODEL, 2 * D_FF), D_MODEL ** -0.5)
    b1 = nrm(ks[14], (DEPTH, N_EXPERTS, 2 * D_FF), 0.01)
    w2 = nrm(ks[15], (DEPTH, N_EXPERTS, D_FF, D_MODEL), DEEPNORM_BETA * D_FF ** -0.5)
    b2 = nrm(ks[16], (DEPTH, N_EXPERTS, D_MODEL), 0.01)
    ln2_g = 1.0 + nrm(ks[17], (DEPTH, D_MODEL), 0.01)
    ln2_b = nrm(ks[18], (DEPTH, D_MODEL), 0.01)
    return {"x": x, "emb_ln_g": emb_ln_g, "emb_ln_b": emb_ln_b, "w_in": w_in, "b_in": b_in,
            "conv_w": conv_w, "ml_norm_w": ml_norm_w, "ret_norm_w": ret_norm_w, "w_out": w_out,
            "ln1_g": ln1_g, "ln1_b": ln1_b, "w_router": w_router, "b_router": b_router,
            "w1": w1, "b1": b1, "w2": w2, "b2": b2, "ln2_g": ln2_g, "ln2_b": ln2_b}


def reference(x, emb_ln_g, emb_ln_b, w_in, b_in, conv_w, ml_norm_w, ret_norm_w, w_out,
              ln1_g, ln1_b, w_router, b_router, w1, b1, w2, b2, ln2_g, ln2_b):
    x = _layernorm(x, emb_ln_g, emb_ln_b)
    for l in range(DEPTH):
        proj = (x @ w_in[l] + b_in[l]).astype(jnp.float32)
        y_f = _fourier_mixer(proj[..., COL_F:COL_MQK])
        qk_m = jax.nn.silu(_centred_dwconv(proj[..., COL_MQK:COL_MV], conv_w[l]))
        y_m = _mlstm_mixer(qk_m, proj[..., COL_MV:COL_MO], proj[..., COL_MO:COL_MG],
                           proj[..., COL_MG:COL_RQ], ml_norm_w[l])
        y_r = _retention_mixer(proj[..., COL_RQ:COL_RK], proj[..., COL_RK:COL_RV],
                               proj[..., COL_RV:COL_RG], proj[..., COL_RG:PROJ_W], ret_norm_w[l])
        mix = jnp.concatenate([y_f, y_m, y_r], axis=-1).astype(x.dtype) @ w_out[l]
        x = _layernorm(DEEPNORM_ALPHA * x + mix, ln1_g[l], ln1_b[l])
        moe = _moe(x, w_router[l], b_router[l], w1[l], b1[l], w2[l], b2[l])
        x = _layernorm(DEEPNORM_ALPHA * x + moe, ln2_g[l], ln2_b[l])
    return x
```

```python
import numpy as np
import concourse.bass as bass
import concourse.mybir as mybir

F32 = mybir.dt.float32
BF16 = mybir.dt.bfloat16
I32 = mybir.dt.int32
U32 = mybir.dt.uint32
AF = mybir.ActivationFunctionType
ALU = mybir.AluOpType
AX = mybir.AxisListType


class Dep:
    __slots__ = ("w", "r", "pw")

    def __init__(self):
        self.w = {}
        self.r = {}
        self.pw = {}


def _merge(dst, src):
    for k, v in src.items():
        if dst.get(k, 0) < v:
            dst[k] = v


class KB:
    def __init__(self, nc, nds=16):
        self.nc = nc
        self.E = {"pe": nc.tensor, "act": nc.scalar, "dve": nc.vector, "pool": nc.gpsimd, "sp": nc.sync}
        self.csem = {e: nc.alloc_semaphore("c_" + e) for e in ("pe", "act", "dve", "pool")}
        self.ccnt = {e: 0 for e in self.csem}
        self.dsem = {q: [nc.alloc_semaphore("d_%s%d" % (q, i)) for i in range(nds)] for q in ("sp", "act", "pool")}
        self.dcnt = {q: [0] * nds for q in self.dsem}
        self.drr = {q: 0 for q in self.dsem}
        self.seen = {e: {} for e in self.E}
        self.ntile = 0
        import contextlib
        self._ES = contextlib.ExitStack
        self.scopes = [self._ES()]

    def push(self):
        self.scopes.append(self._ES())

    def pop(self):
        self.barrier()
        self.scopes.pop().close()

    def sb(self, shape, dtype, name=None):
        self.ntile += 1
        return self.scopes[-1].enter_context(self.nc.sbuf_tensor(name or "t%d" % self.ntile, list(shape), dtype))

    def ps(self, shape, dtype=F32, name=None):
        self.ntile += 1
        return self.scopes[-1].enter_context(self.nc.psum_tensor(name or "p%d" % self.ntile, list(shape), dtype))

    def _sem(self, k):
        return self.csem[k[1]] if k[0] == "c" else self.dsem[k[1]][k[2]]

    def _need(self, e, toks):
        seen = self.seen[e]
        for k, v in toks.items():
            if e == "pe" and k == ("c", "pe"):
                continue
            if seen.get(k, 0) >= v:
                continue
            seen[k] = v
            self.E[e].wait_ge(self._sem(k), v)

    def _deps(self, r, w, wa):
        toks = {}
        for d in r:
            _merge(toks, d.w)
        for d in w:
            _merge(toks, d.w)
            _merge(toks, d.r)
        for d in wa:
            _merge(toks, d.r)
            _merge(toks, d.pw)
        return toks

    def _mark(self, tok, r, w, wa):
        k, v = tok
        for d in r:
            if d.r.get(k, 0) < v:
                d.r[k] = v
        for d in w:
            pw = dict(d.w)
            _merge(pw, d.r)
            d.pw = pw
            d.w = {k: v}
            d.r = {}
        for d in wa:
            if d.w.get(k, 0) < v:
                d.w[k] = v

    def op(self, e, fn, r=(), w=(), wa=(), drain=False):
        if drain and self.ccnt[e]:
            k = ("c", e)
            if self.seen[e].get(k, 0) < self.ccnt[e]:
                self.seen[e][k] = self.ccnt[e]
                self.E[e].wait_ge(self.csem[e], self.ccnt[e])
        self._need(e, self._deps(r, w, wa))
        ins = fn(self.E[e])
        self.ccnt[e] += 1
        ins.then_inc(self.csem[e], 1)
        self._mark((("c", e), self.ccnt[e]), r, w, wa)
        return ins

    def dma(self, q, out, in_, r=(), w=(), wa=(), fn=None, **kw):
        j = self.drr[q]
        self.drr[q] = (j + 1) % len(self.dsem[q])
        toks = self._deps(r, w, wa)
        if self.dcnt[q][j]:
            _merge(toks, {("d", q, j): self.dcnt[q][j]})
        self._need(q, toks)
        if fn is None:
            ins = self.E[q].dma_start(out=out, in_=in_, **kw)
        else:
            ins = fn(self.E[q])
        self.dcnt[q][j] += 16
        ins.then_inc(self.dsem[q][j], 16)
        self._mark((("d", q, j), self.dcnt[q][j]), r, w, wa)
        return ins

    def all_tokens(self):
        toks = {}
        for e, c in self.ccnt.items():
            if c:
                toks[("c", e)] = c
        for q, lst in self.dcnt.items():
            for j, c in enumerate(lst):
                if c:
                    toks[("d", q, j)] = c
        return toks

    def barrier(self, engines=None):
        toks = self.all_tokens()
        for e in (engines or self.E):
            self._need(e, toks)

A3STOP = 99
A3_ND = 2
A3_NN = 64
A3_E3 = 'pool'
A3_FIN = 1
A3_ST = 7

S = 8192
NCH = 64
D = 1024
TM_Z = 0
TM_MG = 256
TM_RQ = 264
TM_RK = 360
TM_MV = 456
TM_MO = 648
TM_RV = 840
TM_RG = 1032
TM_N = 1224
TM_BANKS = ((0, 456), (456, 840), (840, 1224))
RET_G0 = 5.0
RET_BOFF = 0.5


def consts_a(j):
    c = {}
    c["ident_f"] = np.eye(128, dtype=np.float32)
    c["ones_f"] = np.ones((128, 128), np.float32)
    a = np.arange(64, dtype=np.float64)
    ang = 2 * np.pi * np.outer(a, a) / 64.0
    C64, S64 = np.cos(ang), np.sin(ang)
    T = np.zeros((128, 256), np.float64)
    for g in range(2):
        T[g * 64:(g + 1) * 64, g * 64:(g + 1) * 64] = C64
        T[g * 64:(g + 1) * 64, 128 + g * 64:128 + (g + 1) * 64] = -S64
    c["fT"] = T.astype(np.float32)
    c["tabA"] = np.concatenate([C64, -S64], 1).astype(np.float32)
    c["tabB"] = np.concatenate([S64, C64], 1).astype(np.float32)
    p = np.arange(128, dtype=np.float64)
    tw = 2 * np.pi * np.outer(p, a) / 8192.0
    sc = 1.0 / np.sqrt(8192.0 * 64.0)
    c["ct4"] = np.tile((np.cos(tw) * sc)[:, None, :], (1, 4, 1)).reshape(128, 256).astype(np.float32)
    c["st4"] = np.tile((np.sin(tw) * sc)[:, None, :], (1, 4, 1)).reshape(128, 256).astype(np.float32)
    a2 = 2 * np.pi * np.outer(p, p) / 128.0
    c["C128"] = np.cos(a2).astype(np.float32)
    c["S128"] = np.sin(a2).astype(np.float32)
    s_ = np.arange(128)[:, None]; t_ = np.arange(128)[None, :]
    c["maskF"] = (s_ <= t_).astype(np.float32)
    c["maskB"] = (s_ >= t_).astype(np.float32)
    dec = np.zeros((128, 4, 128), np.float64)
    rsc = np.zeros((128, 16), np.float64)
    gch = np.zeros((4,), np.float64)
    for hl in range(2):
        h = 2 * j + hl
        for d in range(2):
            lg = np.log1p(-2.0 ** (-(RET_G0 + (RET_BOFF if d else 0.0)) - h))
            if d == 0:
                m = np.where(s_ <= t_, np.exp(lg * np.maximum(t_ - s_, 0)), 0.0)
                wkey = np.exp(lg * (127 - np.arange(128)))
                wint = np.exp(lg * (np.arange(128) + 1.0))
            else:
                m = np.where(s_ > t_, np.exp(lg * np.maximum(s_ - t_, 0)), 0.0)
                wkey = np.exp(lg * np.arange(128))
                wint = np.exp(lg * (128.0 - np.arange(128)))
            dec[:, hl * 2 + d, :] = m
            rsc[:, (hl * 2 + d) * 4 + 0] = wkey
            rsc[:, (hl * 2 + d) * 4 + 1] = wint
            rsc[:, (hl * 2 + d) * 4 + 2] = np.exp(lg * 128.0)
            gch[hl * 2 + d] = np.exp(lg * 128.0)
    c["rdec"] = dec.reshape(128, 512).astype(np.float32)
    c["rsc"] = rsc.astype(np.float32)
    inv = 1.0 / (10000.0 ** (np.arange(0, 48, 2, dtype=np.float64) / 48.0))
    pos = (np.arange(64)[None, :] * 128 + np.arange(128)[:, None]).astype(np.float64)
    angr = pos[:, :, None] * inv[None, None, :]
    angr = (pos.astype(np.float32)[:, :, None] * inv.astype(np.float32)[None, None, :]).astype(np.float32).astype(np.float64)
    c["rcos"] = np.cos(angr).reshape(128, 64 * 24).astype(np.float32)
    c["rsin"] = np.sin(angr).reshape(128, 64 * 24).astype(np.float32)
    return c, [float(x) for x in gch]


def build_a(nc, kb, io, gch, stages=("a1", "a2", "a3", "a4")):
    ident_f = kb.sb([128, 128], F32); ident_b = kb.sb([128, 128], BF16)
    ones_f = kb.sb([128, 128], F32); ones_b = kb.sb([128, 128], BF16)
    cd = Dep()
    kb.dma("sp", ident_f[:, :], io["ident_f"], w=[cd])
    kb.dma("sp", ones_f[:, :], io["ones_f"], wa=[cd])
    kb.dma("pool", ident_b[:, :], io["ident_f"], wa=[cd])
    kb.dma("pool", ones_b[:, :], io["ones_f"], wa=[cd])
    kb.push()
    G = kb.sb([128, NCH, 8], F32); G_d = Dep()
    qT = kb.sb([128, S], BF16); kT = kb.sb([128, S], BF16); qk_d = Dep()
    tm_d = Dep()
    ym_d = Dep()

    if "a1" in stages:
        kb.push()
        wt = kb.sb([128, 8, TM_N], BF16); wt_d = Dep()
        for c in range(8):
            kb.dma("pool", wt[:, c, 256:TM_N], io["wtm"][c * 128:(c + 1) * 128, :], wa=[wt_d])
        brow = kb.sb([1, TM_N], BF16)
        kb.dma("pool", brow[:, 256:TM_N], io["btm"].rearrange("(o n) -> o n", o=1), wa=[wt_d])
        wq = kb.sb([128, 8, 128], BF16); wk = kb.sb([128, 8, 128], BF16)
        kb.dma("pool", wq[:, :, :], io["wq"].rearrange("(c p) m -> p c m", p=128), wa=[wt_d])
        kb.dma("pool", wk[:, :, :], io["wk"].rearrange("(c p) m -> p c m", p=128), wa=[wt_d])
        bqk = kb.sb([128, 2], F32)
        kb.dma("sp", bqk[:, 0:1], io["bq"].rearrange("(p o) -> p o", o=1), wa=[wt_d])
        kb.dma("sp", bqk[:, 1:2], io["bk"].rearrange("(p o) -> p o", o=1), wa=[wt_d])
        cw = kb.sb([128, 6], F32)
        kb.dma("sp", cw[:, 0:3], io["convq"], wa=[wt_d])
        kb.dma("sp", cw[:, 3:6], io["convk"], wa=[wt_d])
        wfT = kb.sb([128, D], F32); fT = kb.sb([128, 256], F32); bfc = kb.sb([128, 1], F32)
        kb.dma("sp", wfT[:, :], io["wfT"], wa=[wt_d])
        kb.dma("sp", fT[:, :], io["fT"], wa=[wt_d])
        kb.dma("sp", bfc[:, :], io["bf"].rearrange("(p o) -> p o", o=1), wa=[wt_d])
        ps_f = kb.ps([128, 512], F32); ps_f_d = Dep()
        for c in range(8):
            kb.op("pe", lambda e: e.matmul(ps_f[:, 0:256], wfT[:, c * 128:(c + 1) * 128], fT[:, :], start=True, stop=True), r=[wt_d], w=[ps_f_d])
            kb.op("act", lambda e: e.activation(out=wt[:, c, 0:256], in_=ps_f[:, 0:256], func=AF.Copy), r=[ps_f_d], wa=[wt_d])
        kb.op("pe", lambda e: e.matmul(ps_f[0:1, 0:256], bfc[:, 0:1], fT[:, :], start=True, stop=True), r=[wt_d], w=[ps_f_d])
        kb.op("act", lambda e: e.activation(out=brow[0:1, 0:256], in_=ps_f[0:1, 0:256], func=AF.Copy), r=[ps_f_d], wa=[wt_d])

        qpre = kb.sb([128, S + 2], BF16); kpre = kb.sb([128, S + 2], BF16); pre_d = Dep()
        for tl in (qpre, kpre):
            kb.op("dve", lambda e: e.memset(tl[:, 0:1], 0.0), wa=[pre_d])
            kb.op("dve", lambda e: e.memset(tl[:, S + 1:S + 2], 0.0), wa=[pre_d])
        xt = [kb.sb([128, 8, 512], BF16) for _ in range(2)]; xt_d = [Dep() for _ in range(2)]
        stg = [kb.sb([128, 4, TM_N], BF16) for _ in range(2)]; stg_d = [Dep() for _ in range(2)]
        ps_q = kb.ps([128, 512], F32); ps_q_d = Dep()
        ps_k = kb.ps([128, 512], F32); ps_k_d = Dep()
        ps_t = [kb.ps([128, 3, 512], F32) for _ in range(1)]; ps_t_d = [[Dep() for _ in range(3)] for _ in range(1)]
        for g in range(16):
            b = g % 2
            kb.dma("sp", xt[b][:, :, :], io["xT"][:, g * 512:(g + 1) * 512].rearrange("(c p) t -> p c t", p=128), w=[xt_d[b]])
            for (pst, pd, wgt, col, dst) in ((ps_q, ps_q_d, wq, 0, qpre), (ps_k, ps_k_d, wk, 1, kpre)):
                for c in range(8):
                    kb.op("pe", lambda e: e.matmul(pst[:, :], wgt[:, c, :], xt[b][:, c, :], start=(c == 0), stop=(c == 7)),
                          r=[xt_d[b], wt_d], w=[pd] if c == 0 else (), wa=() if c == 0 else [pd])
                kb.op("act", lambda e: e.activation(out=dst[:, 1 + g * 512:1 + (g + 1) * 512], in_=pst[:, :], func=AF.Identity, bias=bqk[:, col:col + 1]),
                      r=[pd, wt_d], wa=[pre_d])
            for sub in range(4):
                pb = 0
                for bi, (c0, c1) in enumerate(TM_BANKS):
                    pd = ps_t_d[pb][bi]
                    kb.op("pe", lambda e: e.matmul(ps_t[pb][:, bi, 0:c1 - c0], ones_b[0:1, :], brow[0:1, c0:c1], start=True, stop=False), r=[wt_d, cd], w=[pd])
                    for c in range(8):
                        kb.op("pe", lambda e: e.matmul(ps_t[pb][:, bi, 0:c1 - c0], xt[b][:, c, sub * 128:(sub + 1) * 128], wt[:, c, c0:c1],
                                                       start=False, stop=(c == 7)), r=[xt_d[b], wt_d], wa=[pd])
                    eng = "act" if bi != 1 else "dve"
                    if eng == "act":
                        kb.op("act", lambda e: e.activation(out=stg[b][:, sub, c0:c1], in_=ps_t[pb][:, bi, 0:c1 - c0], func=AF.Copy), r=[pd],
                              w=[stg_d[b]] if (sub == 0 and bi == 0) else (), wa=() if (sub == 0 and bi == 0) else [stg_d[b]])
                    else:
                        kb.op("dve", lambda e: e.tensor_copy(out=stg[b][:, sub, c0:c1], in_=ps_t[pb][:, bi, 0:c1 - c0]), r=[pd], wa=[stg_d[b]])
                    if bi == 0:
                        kb.op("dve", lambda e: e.tensor_copy(out=G[:, g * 4 + sub, :], in_=ps_t[pb][:, 0, TM_MG:TM_MG + 8]), r=[pd], wa=[G_d])
            kb.dma("sp", io["tm"][g * 512:(g + 1) * 512, :].rearrange("(s p) c -> p s c", p=128), stg[b][:, :, :], r=[stg_d[b]], wa=[tm_d])
        tmpc = kb.sb([128, S], F32); tmpc_d = Dep()
        for (src, dst, o) in ((qpre, qT, 0), (kpre, kT, 3)):
            kb.op("dve", lambda e: e.tensor_scalar(out=tmpc[:, :], in0=src[:, 0:S], scalar1=cw[:, o:o + 1], scalar2=None, op0=ALU.mult), r=[pre_d, wt_d], w=[tmpc_d])
            kb.op("dve", lambda e: e.scalar_tensor_tensor(out=tmpc[:, :], in0=src[:, 1:S + 1], scalar=cw[:, o + 1:o + 2], in1=tmpc[:, :], op0=ALU.mult, op1=ALU.add), r=[pre_d], w=[tmpc_d])
            kb.op("dve", lambda e: e.scalar_tensor_tensor(out=tmpc[:, :], in0=src[:, 2:S + 2], scalar=cw[:, o + 2:o + 3], in1=tmpc[:, :], op0=ALU.mult, op1=ALU.add), r=[pre_d], w=[tmpc_d])
            if o == 0:
                kb.op("act", lambda e: e.activation(out=dst[:, :], in_=tmpc[:, :], func=AF.Silu), r=[tmpc_d], wa=[qk_d])
            else:
                kb.op("act", lambda e: e.activation(out=tmpc[:, :], in_=tmpc[:, :], func=AF.Silu), r=[tmpc_d], w=[tmpc_d])
                kb.op("dve", lambda e: e.tensor_scalar(out=dst[:, :], in0=tmpc[:, :], scalar1=48.0 ** -0.5, scalar2=None, op0=ALU.mult), r=[tmpc_d], wa=[qk_d])
        kb.pop()

    if "a2" in stages:
        kb.push()
        ZT = kb.sb([64, 128, 256], BF16); zt_d = Dep()
        for q4 in range(4):
            kb.dma("sp", ZT[:, q4 * 32:(q4 + 1) * 32, :], io["tm"].rearrange("(t p) c -> t p c", p=128)[:, q4 * 32:(q4 + 1) * 32, 0:256], r=[tm_d], wa=[zt_d])
        tabA = kb.sb([64, 128], BF16); tabB = kb.sb([64, 128], BF16); C128 = kb.sb([128, 128], BF16); S128 = kb.sb([128, 128], BF16)
        ct4 = kb.sb([128, 256], F32); st4 = kb.sb([128, 256], F32); fc_d = Dep()
        kb.dma("pool", tabA[:, :], io["tabA"], wa=[fc_d]); kb.dma("pool", tabB[:, :], io["tabB"], wa=[fc_d])
        kb.dma("pool", C128[:, :], io["C128"], wa=[fc_d]); kb.dma("pool", S128[:, :], io["S128"], wa=[fc_d])
        kb.dma("sp", ct4[:, :], io["ct4"], wa=[fc_d]); kb.dma("sp", st4[:, :], io["st4"], wa=[fc_d])
        H = kb.sb([128, 2, 128, 64], BF16); H_d = Dep()
        yfT = kb.sb([128, S], BF16); yf_d = Dep()
        psG = [kb.ps([128, 4, 128], F32) for _ in range(2)]; psG_d = [Dep() for _ in range(2)]
        Gs = [kb.sb([128, 4, 128], F32) for _ in range(2)]; Gs_d = [Dep() for _ in range(2)]
        t1 = [kb.sb([128, 4, 64], F32) for _ in range(2)]; t2 = [kb.sb([128, 4, 64], F32) for _ in range(2)]
        t3 = [kb.sb([128, 4, 64], F32) for _ in range(2)]; t4 = [kb.sb([128, 4, 64], F32) for _ in range(2)]
        ta_d = [Dep() for _ in range(2)]; tb_d = [Dep() for _ in range(2)]
        ct3 = ct4[:, :].rearrange("p (c k) -> p c k", c=4); st3 = st4[:, :].rearrange("p (c k) -> p c k", c=4)
        for cg in range(32):
            b = cg % 2
            for ci in range(4):
                c = cg * 4 + ci
                kb.op("pe", lambda e: e.matmul(psG[b][:, ci, :], ZT[:, :, c], tabA[:, :], start=True, stop=False), r=[zt_d, fc_d],
                      w=[psG_d[b]] if ci == 0 else (), wa=() if ci == 0 else [psG_d[b]])
                kb.op("pe", lambda e: e.matmul(psG[b][:, ci, :], ZT[:, :, 128 + c], tabB[:, :], start=False, stop=True), r=[zt_d, fc_d], wa=[psG_d[b]])
            kb.op("act", lambda e: e.activation(out=Gs[b][:, :, :], in_=psG[b][:, :, :], func=AF.Copy), r=[psG_d[b]], w=[Gs_d[b]])
            Gr = Gs[b][:, :, 0:64]; Gi = Gs[b][:, :, 64:128]
            kb.op("dve", lambda e: e.tensor_tensor(out=t1[b][:, :, :], in0=Gr, in1=ct3, op=ALU.mult), r=[Gs_d[b], fc_d], w=[ta_d[b]])
            kb.op("dve", lambda e: e.tensor_tensor(out=t2[b][:, :, :], in0=Gi, in1=st3, op=ALU.mult), r=[Gs_d[b]], wa=[ta_d[b]])
            kb.op("dve", lambda e: e.tensor_tensor(out=H[:, 0, cg * 4:(cg + 1) * 4, :], in0=t1[b][:, :, :], in1=t2[b][:, :, :], op=ALU.add), r=[ta_d[b]], wa=[H_d])
            kb.op("pool", lambda e: e.tensor_tensor(out=t3[b][:, :, :], in0=Gi, in1=ct3, op=ALU.mult), r=[Gs_d[b], fc_d], w=[tb_d[b]])
            kb.op("pool", lambda e: e.tensor_tensor(out=t4[b][:, :, :], in0=Gr, in1=st3, op=ALU.mult), r=[Gs_d[b]], wa=[tb_d[b]])
            kb.op("pool", lambda e: e.tensor_tensor(out=H[:, 1, cg * 4:(cg + 1) * 4, :], in0=t3[b][:, :, :], in1=t4[b][:, :, :], op=ALU.subtract), r=[tb_d[b]], wa=[H_d])
        yf3 = yfT[:, :].rearrange("c (k2 k1) -> c k1 k2", k1=64)
        for kg in range(16):
            b = kg % 2
            for ki in range(4):
                k1 = kg * 4 + ki
                kb.op("pe", lambda e: e.matmul(psG[b][:, ki, :], H[:, 0, :, k1], C128[:, :], start=True, stop=False), r=[H_d, fc_d],
                      w=[psG_d[b]] if ki == 0 else (), wa=() if ki == 0 else [psG_d[b]])
                kb.op("pe", lambda e: e.matmul(psG[b][:, ki, :], H[:, 1, :, k1], S128[:, :], start=False, stop=True), r=[H_d, fc_d], wa=[psG_d[b]])
            kb.op("act", lambda e: e.activation(out=yf3[:, kg * 4:(kg + 1) * 4, :], in_=psG[b][:, :, :], func=AF.Copy), r=[psG_d[b]], wa=[yf_d])
        kb.dma("sp", io["ymT"][0:128, :], yfT[:, :], r=[yf_d], wa=[ym_d])
        kb.pop()

    def head_norm_fin(src, src_d, nw, gate_sig, hn, out_bf, out_d):
        st, mv, rs, tmpn, hn_d = hn
        for hl in range(2):
            kb.op("dve", lambda e: e.bn_stats(out=st[:, hl, :], in_=src[:, hl * 96:(hl + 1) * 96]), r=[src_d], w=[hn_d] if hl == 0 else (), wa=() if hl == 0 else [hn_d])
        for hl in range(2):
            kb.op("dve", lambda e: e.bn_aggr(out=mv[:, hl * 2:hl * 2 + 2], in_=st[:, hl, :]), r=[hn_d], wa=[hn_d])
        kb.op("act", lambda e: e.activation(out=rs[:, 0:2], in_=mv[:, 1:4:2], func=AF.Ln, bias=1e-5), r=[hn_d], wa=[hn_d])
        kb.op("act", lambda e: e.activation(out=rs[:, 0:2], in_=rs[:, 0:2], func=AF.Exp, scale=-0.5), r=[hn_d], wa=[hn_d])
        for hl in range(2):
            kb.op("dve", lambda e: e.tensor_scalar(out=tmpn[:, hl * 96:(hl + 1) * 96], in0=src[:, hl * 96:(hl + 1) * 96], scalar1=mv[:, hl * 2:hl * 2 + 1],
                                                   scalar2=rs[:, hl:hl + 1], op0=ALU.subtract, op1=ALU.mult), r=[src_d, hn_d], wa=[hn_d])
        if gate_sig is None:
            kb.op("dve", lambda e: e.tensor_tensor(out=out_bf, in0=tmpn[:, :], in1=nw, op=ALU.mult), r=[hn_d], w=[out_d])
        else:
            gt, gt_d = gate_sig
            kb.op("dve", lambda e: e.tensor_tensor(out=tmpn[:, :], in0=tmpn[:, :], in1=nw, op=ALU.mult), r=[hn_d], wa=[hn_d])
            kb.op("dve", lambda e: e.tensor_tensor(out=out_bf, in0=tmpn[:, :], in1=gt, op=ALU.mult), r=[hn_d, gt_d], w=[out_d])

    def sigmoid_of(dst, src, rd, wd, n):
        kb.op("act", lambda e: e.activation(out=dst, in_=src, func=AF.Exp, scale=-1.0), r=rd, w=[wd])
        kb.op("dve", lambda e: e.tensor_scalar(out=dst, in0=dst, scalar1=1.0, scalar2=None, op0=ALU.add), r=[wd], w=[wd])
        kb.op("dve", lambda e: e.reciprocal(out=dst, in_=dst), r=[wd], w=[wd])

    def emit_out(ytok_bf, ytok_d, ymS, ymS_d, n, ps_o, ps_o_d):
        for hl in range(2):
            kb.op("pe", lambda e: e.transpose(ps_o[0:96, hl * 128:(hl + 1) * 128], ytok_bf[:, hl * 96:(hl + 1) * 96], ident_b[:, :]), r=[ytok_d, cd],
                  w=[ps_o_d] if hl == 0 else (), wa=() if hl == 0 else [ps_o_d])
        for hl in range(2):
            kb.op("act", lambda e: e.activation(out=ymS[hl][:, n * 128:(n + 1) * 128], in_=ps_o[0:96, hl * 128:(hl + 1) * 128], func=AF.Copy), r=[ps_o_d], wa=[ymS_d])


    def sigmoid_inplace(T, T_d, mul_by_self):
        scr = kb.sb([128, 16 * 192], F32); scr_d = Dep()
        for q in range(4):
            src = T[:, q * 16:(q + 1) * 16, :].rearrange("p n c -> p (n c)")
            kb.op("act", lambda e: e.activation(out=scr[:, :], in_=src, func=AF.Exp, scale=-1.0), r=[T_d], w=[scr_d])
            kb.op("dve", lambda e: e.tensor_scalar(out=scr[:, :], in0=scr[:, :], scalar1=1.0, scalar2=None, op0=ALU.add), r=[scr_d], w=[scr_d])
            kb.op("dve", lambda e: e.reciprocal(out=scr[:, :], in_=scr[:, :]), r=[scr_d], w=[scr_d])
            if mul_by_self:
                kb.op("dve", lambda e: e.tensor_tensor(out=src, in0=src, in1=scr[:, :], op=ALU.mult), r=[scr_d, T_d], w=[T_d])
            else:
                kb.op("dve", lambda e: e.tensor_copy(out=src, in_=scr[:, :]), r=[scr_d, T_d], w=[T_d])

    def finalize_batched(hsum, hs_d, GT, GT_d, nw, row0, ps_o, ps_o_d):
        pre_gate = (row0 == 128)
        HN = 16
        hg = kb.sb([128, HN, 192], F32); hg_d = Dep()
        sq = kb.sb([128, HN, 192], F32); sq_d = Dep()
        st1 = kb.sb([128, HN * 2], F32); st2 = kb.sb([128, HN * 2], F32); st_d = Dep()
        yt = kb.sb([128, HN, 192], BF16); yt_d = Dep()
        stg = [[kb.sb([96, 512], BF16) for _ in range(2)] for _ in range(2)]; stg_d = [[Dep() for _ in range(2)] for _ in range(2)]
        g3 = lambda t: t[:, :, :].rearrange("p n (h c) -> p (n h) c", h=2)
        f2_ = lambda t: t[:, :, :].rearrange("p n c -> p (n c)")
        for q in range(NCH // HN):
            nsl = slice(q * HN, (q + 1) * HN)
            hq = hsum[:, nsl, :].rearrange("p n c -> p (n c)")
            gq = GT[:, nsl, :].rearrange("p n c -> p (n c)")
            hdeps = [hs_d[n] for n in range(q * HN, (q + 1) * HN)]
            if pre_gate:
                kb.op("dve", lambda e: e.tensor_tensor(out=f2_(hg), in0=hq, in1=gq, op=ALU.mult), r=hdeps + [GT_d], w=[hg_d])
            else:
                kb.op("dve", lambda e: e.tensor_copy(out=f2_(hg), in_=hq), r=hdeps, w=[hg_d])
            kb.op("pool", lambda e: e.tensor_tensor(out=f2_(sq), in0=f2_(hg), in1=f2_(hg), op=ALU.mult), r=[hg_d], w=[sq_d])
            kb.op("dve", lambda e: e.reduce_sum(out=st1[:, :], in_=g3(hg), axis=AX.X), r=[hg_d], w=[st_d])
            kb.op("dve", lambda e: e.reduce_sum(out=st2[:, :], in_=g3(sq), axis=AX.X), r=[sq_d, st_d], w=[st_d])
            S_ = lambda fn, eng="dve": kb.op(eng, fn, r=[st_d], w=[st_d])
            S_(lambda e: e.tensor_scalar(out=st1[:, :], in0=st1[:, :], scalar1=1.0 / 96.0, scalar2=None, op0=ALU.mult))
            S_(lambda e: e.tensor_scalar(out=st2[:, :], in0=st2[:, :], scalar1=1.0 / 96.0, scalar2=None, op0=ALU.mult))
            S_(lambda e: e.tensor_tensor(out=sq[:, 0, 0:HN * 2], in0=st1[:, :], in1=st1[:, :], op=ALU.mult))
            kb.op("dve", lambda e: e.tensor_tensor(out=st2[:, :], in0=st2[:, :], in1=sq[:, 0, 0:HN * 2], op=ALU.subtract), r=[st_d, sq_d], w=[st_d])
            S_(lambda e: e.activation(out=st2[:, :], in_=st2[:, :], func=AF.Ln, bias=1e-5), "act")
            S_(lambda e: e.activation(out=st2[:, :], in_=st2[:, :], func=AF.Exp, scale=-0.5), "act")
            mean_b = st1[:, :].unsqueeze(2).broadcast_to([128, HN * 2, 96])
            rstd_b = st2[:, :].unsqueeze(2).broadcast_to([128, HN * 2, 96])
            nw_b = nw[:, :].rearrange("p (h c) -> p h c", h=2).unsqueeze(1).broadcast_to([128, HN, 2, 96])
            kb.op("dve", lambda e: e.tensor_tensor(out=g3(hg), in0=g3(hg), in1=mean_b, op=ALU.subtract), r=[hg_d, st_d], w=[hg_d])
            kb.op("dve", lambda e: e.tensor_tensor(out=g3(hg), in0=g3(hg), in1=rstd_b, op=ALU.mult), r=[hg_d, st_d], w=[hg_d])
            hg4 = hg[:, :, :].rearrange("p n (h c) -> p n h c", h=2)
            if pre_gate:
                kb.op("dve", lambda e: e.tensor_tensor(out=yt[:, :, :].rearrange("p n (h c) -> p n h c", h=2), in0=hg4, in1=nw_b, op=ALU.mult), r=[hg_d], w=[yt_d])
            else:
                kb.op("dve", lambda e: e.tensor_tensor(out=hg4, in0=hg4, in1=nw_b, op=ALU.mult), r=[hg_d], w=[hg_d])
                kb.op("dve", lambda e: e.tensor_tensor(out=f2_(yt), in0=f2_(hg), in1=gq, op=ALU.mult), r=[hg_d, GT_d], w=[yt_d])
            for n4 in range(HN // 4):
                sb_ = (q * (HN // 4) + n4) % 2
                for i4 in range(4):
                    for hl in range(2):
                        kb.op("pe", lambda e: e.transpose(ps_o[0:96, (hl * 4 + i4) * 128:(hl * 4 + i4 + 1) * 128], yt[:, n4 * 4 + i4, hl * 96:(hl + 1) * 96], ident_b[:, :]),
                              r=[yt_d, cd], w=[ps_o_d] if (i4 == 0 and hl == 0) else (), wa=() if (i4 == 0 and hl == 0) else [ps_o_d])
                for hl in range(2):
                    kb.op("act", lambda e: e.activation(out=stg[hl][sb_][:, :], in_=ps_o[0:96, hl * 512:(hl + 1) * 512], func=AF.Copy), r=[ps_o_d], w=[stg_d[hl][sb_]])
                    t0_ = (q * HN + n4 * 4) * 128
                    kb.dma("sp", io["ymT"][row0 + hl * 96:row0 + (hl + 1) * 96, t0_:t0_ + 512], stg[hl][sb_][:, :], r=[stg_d[hl][sb_]], wa=[ym_d])

    tmv = io["tm"].rearrange("(n p) c -> p n c", p=128)

    if "a3" in stages:
        kb.push()
        maskF = kb.sb([128, 128], F32); maskB = kb.sb([128, 128], F32); mk_d = Dep()
        kb.dma("sp", maskF[:, :], io["maskF"], wa=[mk_d]); kb.dma("sp", maskB[:, :], io["maskB"], wa=[mk_d])
        nw = kb.sb([128, 192], F32)
        kb.dma("sp", nw[:, :], io["mnw"].partition_broadcast(128), wa=[mk_d])
        V = kb.sb([128, NCH, 192], BF16); O = kb.sb([128, NCH, 192], BF16); vo_d = Dep()
        for q8 in range(8):
            kb.dma("sp", V[:, q8 * 8:(q8 + 1) * 8, :], tmv[:, q8 * 8:(q8 + 1) * 8, TM_MV:TM_MV + 192], r=[tm_d], wa=[vo_d])
            kb.dma("sp", O[:, q8 * 8:(q8 + 1) * 8, :], tmv[:, q8 * 8:(q8 + 1) * 8, TM_MO:TM_MO + 192], r=[tm_d], wa=[vo_d])
        sm = {nm: kb.sb([128, NCH, 4], F32) for nm in ("Fp", "Ip", "lf", "a", "Aend", "b", "BM", "mch", "mnew", "mprev", "sprev", "scur", "M", "eM", "p", "fa", "fi", "t0")}
        smd = Dep()
        f2 = lambda nm: sm[nm][:, :, :].rearrange("p n c -> p (n c)")
        for d in range(2):
            kb.op("dve", lambda e: e.tensor_copy(out=sm["Ip"][:, :, d * 2:d * 2 + 2], in_=G[:, :, d * 4:d * 4 + 2]), r=[G_d], wa=[smd])
            kb.op("dve", lambda e: e.tensor_copy(out=sm["Fp"][:, :, d * 2:d * 2 + 2], in_=G[:, :, d * 4 + 2:d * 4 + 4]), r=[G_d], wa=[smd])
        S1 = lambda fn, eng="dve": kb.op(eng, fn, r=[smd], w=[smd])
        S1(lambda e: e.activation(out=f2("lf"), in_=f2("Fp"), func=AF.Exp, scale=-1.0), "act")
        S1(lambda e: e.activation(out=f2("lf"), in_=f2("lf"), func=AF.Ln, bias=1.0), "act")
        S1(lambda e: e.tensor_scalar(out=f2("lf"), in0=f2("lf"), scalar1=-1.0, scalar2=None, op0=ALU.mult))
        if A3STOP == 10:
            kb.pop(); kb.barrier(); return
        ps_s = kb.ps([128, 2, 512], F32); ps_s_d = Dep()
        for d in range(2):
            kb.op("pe", lambda e: e.matmul(ps_s[:, 0, d * 128:(d + 1) * 128], (maskF if d == 0 else maskB)[:, :], sm["lf"][:, :, d * 2:d * 2 + 2], start=True, stop=True),
                  r=[smd, mk_d], w=[ps_s_d] if d == 0 else (), wa=() if d == 0 else [ps_s_d])
        kb.op("pe", lambda e: e.matmul(ps_s[:, 1, 0:256], ones_f[:, :], f2("lf"), start=True, stop=True), r=[smd, cd], wa=[ps_s_d])
        for d in range(2):
            kb.op("dve", lambda e: e.tensor_copy(out=sm["a"][:, :, d * 2:d * 2 + 2], in_=ps_s[:, 0, d * 128:(d + 1) * 128].rearrange("p (n h) -> p n h", h=2)), r=[ps_s_d, smd], w=[smd])
        kb.op("dve", lambda e: e.tensor_copy(out=f2("Aend"), in_=ps_s[:, 1, 0:256]), r=[ps_s_d, smd], w=[smd])
        S1(lambda e: e.tensor_tensor(out=f2("b"), in0=f2("Ip"), in1=f2("a"), op=ALU.subtract))
        if A3STOP == 11:
            kb.pop(); kb.barrier(); return
        bmc = kb.sb([128, 2], F32); dg = kb.sb([128, 128], F32)
        for hf in range(2):
            kb.op("pe", lambda e: e.transpose(ps_s[:, 0, 0:128], f2("b")[:, hf * 128:(hf + 1) * 128], ident_f[:, :]), r=[smd, cd], w=[ps_s_d])
            kb.op("dve", lambda e: e.reduce_max(out=bmc[:, hf:hf + 1], in_=ps_s[:, 0, 0:128], axis=AX.X), r=[ps_s_d, smd], w=[smd])
            kb.op("dve", lambda e: e.tensor_scalar(out=dg[:, :], in0=ident_f[:, :], scalar1=bmc[:, hf:hf + 1], scalar2=None, op0=ALU.mult), r=[smd, cd], w=[smd])
            kb.op("pe", lambda e: e.matmul(ps_s[:, 1, 0:128], ones_f[:, :], dg[:, :], start=True, stop=True), r=[smd, cd], w=[ps_s_d])
            kb.op("dve", lambda e: e.tensor_copy(out=f2("BM")[:, hf * 128:(hf + 1) * 128], in_=ps_s[:, 1, 0:128]), r=[ps_s_d, smd], w=[smd])
        if A3STOP == 12:
            kb.pop(); kb.barrier(); return
        S1(lambda e: e.tensor_tensor(out=f2("mch"), in0=f2("Aend"), in1=f2("BM"), op=ALU.add))
        for c in range(4):
            sl = (lambda nm: sm[nm][:, :, c]) if c < 2 else (lambda nm: sm[nm][:, ::-1, c])
            S1(lambda e: e.tensor_tensor_scan(out=sl("mnew"), data0=sl("Aend"), data1=sl("mch"), initial=0.0, op0=ALU.add, op1=ALU.max))
        if A3STOP == 13:
            kb.pop(); kb.barrier(); return
        S1(lambda e: e.memset(f2("mprev"), 0.0))
        S1(lambda e: e.tensor_copy(out=sm["mprev"][:, 1:NCH, 0:2], in_=sm["mnew"][:, 0:NCH - 1, 0:2]))
        S1(lambda e: e.tensor_copy(out=sm["mprev"][:, 0:NCH - 1, 2:4], in_=sm["mnew"][:, 1:NCH, 2:4]))
        if A3STOP == 14:
            kb.pop(); kb.barrier(); return
        S1(lambda e: e.tensor_tensor(out=f2("t0"), in0=f2("Aend"), in1=f2("mprev"), op=ALU.add))
        S1(lambda e: e.tensor_tensor(out=f2("sprev"), in0=f2("t0"), in1=f2("mnew"), op=ALU.subtract))
        S1(lambda e: e.activation(out=f2("sprev"), in_=f2("sprev"), func=AF.Exp), "act")
        S1(lambda e: e.tensor_tensor(out=f2("scur"), in0=f2("mch"), in1=f2("mnew"), op=ALU.subtract))
        S1(lambda e: e.activation(out=f2("scur"), in_=f2("scur"), func=AF.Exp), "act")
        if A3STOP == 15:
            kb.pop(); kb.barrier(); return
        S1(lambda e: e.tensor_tensor(out=f2("M"), in0=f2("mprev"), in1=f2("BM"), op=ALU.max))
        S1(lambda e: e.activation(out=f2("eM"), in_=f2("M"), func=AF.Exp, scale=-1.0), "act")
        S1(lambda e: e.tensor_tensor(out=f2("p"), in0=f2("b"), in1=f2("BM"), op=ALU.subtract))
        S1(lambda e: e.activation(out=f2("p"), in_=f2("p"), func=AF.Exp), "act")
        S1(lambda e: e.tensor_tensor(out=f2("t0"), in0=f2("a"), in1=f2("M"), op=ALU.subtract))
        S1(lambda e: e.tensor_tensor(out=f2("fa"), in0=f2("t0"), in1=f2("BM"), op=ALU.add))
        S1(lambda e: e.activation(out=f2("fa"), in_=f2("fa"), func=AF.Exp), "act")
        S1(lambda e: e.tensor_tensor(out=f2("fi"), in0=f2("t0"), in1=f2("mprev"), op=ALU.add))
        S1(lambda e: e.activation(out=f2("fi"), in_=f2("fi"), func=AF.Exp), "act")
        if A3STOP == 1:
            kb.pop(); kb.barrier(); return
        ktok = kb.sb([128, NCH, 128], BF16); ktok_d = Dep()
        ps_o = kb.ps([128, 1024], BF16); ps_o_d = Dep()
        for n4 in range(NCH // 4):
            for q4 in range(4):
                n = n4 * 4 + q4
                kb.op("pe", lambda e: e.transpose(ps_o[:, q4 * 128:(q4 + 1) * 128], kT[:, n * 128:(n + 1) * 128], ident_b[:, :]), r=[qk_d, cd],
                      w=[ps_o_d] if q4 == 0 else (), wa=() if q4 == 0 else [ps_o_d])
            kb.op("act", lambda e: e.activation(out=ktok[:, n4 * 4:(n4 + 1) * 4, :].rearrange("p n c -> p (n c)"), in_=ps_o[:, 0:512], func=AF.Copy), r=[ps_o_d], wa=[ktok_d])
        if A3STOP == 2:
            kb.pop(); kb.barrier(); return
        hsum = kb.sb([128, NCH, 192], BF16); hs_d = [Dep() for _ in range(NCH)]
        sigmoid_inplace(O, vo_d, mul_by_self=False)
        Cm = [kb.sb([128, 98], F32) for _ in range(4)]; Cb = [[kb.sb([128, 98], BF16) for _ in range(2)] for _ in range(4)]
        Cm_d = [Dep() for _ in range(4)]; Cb_d = [[Dep() for _ in range(2)] for _ in range(4)]
        for c in range(4):
            kb.op("dve", lambda e: e.memset(Cm[c][:, :], 0.0), w=[Cm_d[c]])
            kb.op("dve", lambda e: e.memset(Cb[c][0][:, :], 0.0), w=[Cb_d[c][0]])
        ps_st = kb.ps([128, 512], F32); ps_st_d = Dep()
        ps_io = [kb.ps([128, 512], F32) for _ in range(2)]; ps_io_d = [Dep() for _ in range(2)]
        STd = [kb.sb([128, 128], BF16) for _ in range(2)]; STd_d = [Dep() for _ in range(2)]
        vaug = [kb.sb([128, 98], BF16) for _ in range(2)]; vaug_d = [Dep() for _ in range(2)]
        for t_ in vaug:
            kb.op("dve", lambda e: e.memset(t_[:, :], 0.0), w=[vaug_d[vaug.index(t_)]])
        o1 = [kb.sb([128, 98], F32) for _ in range(2)]; o1_d = [Dep() for _ in range(2)]
        dn = [kb.sb([128, 2], F32) for _ in range(2)]
        tcc = [kb.sb([128, 98], F32) for _ in range(2)]; tcc_d = [Dep() for _ in range(2)]
        sgo = kb.sb([128, 192], F32); sgo_d = Dep()
        hfin = kb.sb([128, 192], F32); hfin_d = Dep()
        ytok = [kb.sb([128, 192], BF16) for _ in range(2)]; ytok_d = [Dep() for _ in range(2)]
        hn = (kb.sb([128, 2, 6], F32), kb.sb([128, 4], F32), kb.sb([128, 2], F32), kb.sb([128, 192], F32), Dep())
        it = 0
        for d in range(A3_ND):
            order = range(NCH) if d == 0 else range(NCH - 1, -1, -1)
            msk = maskF if d == 0 else maskB
            for step, n in enumerate(list(order)[:A3_NN]):
                ncol = slice(n * 128, (n + 1) * 128)
                for hl in range(2):
                    c = d * 2 + hl
                    b0 = 64 * hl
                    pb = it % 2; it += 1
                    par = step % 2
                    kb.op("pe", lambda e: e.matmul(ps_st[:, 0:128], kT[b0:b0 + 48, ncol], qT[b0:b0 + 48, ncol], start=True, stop=True), r=[qk_d], w=[ps_st_d], drain=True)
                    kb.op("dve", lambda e: e.tensor_tensor(out=STd[pb][:, :], in0=ps_st[:, 0:128], in1=msk[:, :], op=ALU.mult), r=[ps_st_d, mk_d], w=[STd_d[pb]])
                    kb.op("dve", lambda e: e.tensor_scalar(out=vaug[pb][:, 0:96], in0=V[:, n, hl * 96:(hl + 1) * 96], scalar1=sm["p"][:, n, c:c + 1], scalar2=None, op0=ALU.mult),
                          r=[vo_d, smd], w=[vaug_d[pb]])
                    kb.op("pool", lambda e: e.tensor_copy(out=vaug[pb][:, 96:97], in_=sm["p"][:, n, c:c + 1]), r=[smd], wa=[vaug_d[pb]])
                    P = ps_io[pb]
                    kb.op("pe", lambda e: e.matmul(P[:, 0:98], STd[pb][:, :], vaug[pb][:, :], start=True, stop=True), r=[STd_d[pb], vaug_d[pb]], w=[ps_io_d[pb]])
                    kb.op("pe", lambda e: e.matmul(P[:, 128:226], qT[b0:b0 + 48, ncol], Cb[c][par][b0:b0 + 48, :], start=True, stop=True), r=[qk_d, Cb_d[c][par]], wa=[ps_io_d[pb]])
                    kb.op("pe", lambda e: e.matmul(P[0:b0 + 48, 256:354], ktok[:, n, 0:b0 + 48], vaug[pb][:, :], start=True, stop=True), r=[ktok_d, vaug_d[pb]], wa=[ps_io_d[pb]])
                    kb.op("dve", lambda e: e.tensor_scalar(out=o1[pb][:, :], in0=P[:, 0:98], scalar1=sm["fa"][:, n, c:c + 1], scalar2=None, op0=ALU.mult), r=[ps_io_d[pb], smd], w=[o1_d[pb]])
                    kb.op("dve", lambda e: e.scalar_tensor_tensor(out=o1[pb][:, :], in0=P[:, 128:226], scalar=sm["fi"][:, n, c:c + 1], in1=o1[pb][:, :], op0=ALU.mult, op1=ALU.add),
                          r=[ps_io_d[pb], smd], w=[o1_d[pb]])
                    kb.op("dve", lambda e: e.scalar_tensor_tensor(out=dn[pb][:, 0:1], in0=o1[pb][:, 96:97], scalar=-1.0, in1=o1[pb][:, 96:97], op0=ALU.mult, op1=ALU.max), r=[o1_d[pb]], w=[o1_d[pb]])
                    kb.op("dve", lambda e: e.tensor_tensor(out=dn[pb][:, 0:1], in0=dn[pb][:, 0:1], in1=sm["eM"][:, n, c:c + 1], op=ALU.max), r=[o1_d[pb], smd], w=[o1_d[pb]])
                    kb.op("dve", lambda e: e.reciprocal(out=dn[pb][:, 1:2], in_=dn[pb][:, 0:1]), r=[o1_d[pb]], w=[o1_d[pb]])
                    hdst = hsum[:, n, hl * 96:(hl + 1) * 96]
                    if d == 0:
                        kb.op("dve", lambda e: e.tensor_scalar(out=hdst, in0=o1[pb][:, 0:96], scalar1=dn[pb][:, 1:2], scalar2=None, op0=ALU.mult), r=[o1_d[pb]],
                              w=[hs_d[n]] if hl == 0 else (), wa=() if hl == 0 else [hs_d[n]])
                    else:
                        kb.op("dve", lambda e: e.scalar_tensor_tensor(out=hdst, in0=o1[pb][:, 0:96], scalar=dn[pb][:, 1:2], in1=hdst, op0=ALU.mult, op1=ALU.add), r=[o1_d[pb], hs_d[n]], w=[hs_d[n]])
                    rows = slice(b0, b0 + 48)
                    if (A3_ST & 8) and hl == 1:
                        continue
                    if A3_ST & 1:
                        kb.op("dve", lambda e: e.tensor_scalar(out=tcc[pb][rows, :], in0=P[rows, 256:354], scalar1=sm["scur"][rows, n, c:c + 1], scalar2=None, op0=ALU.mult), r=[ps_io_d[pb], smd], w=[tcc_d[pb]])
                    if A3_ST & 2:
                        kb.op("dve", lambda e: e.scalar_tensor_tensor(out=Cm[c][rows, :], in0=Cm[c][rows, :], scalar=sm["sprev"][rows, n, c:c + 1], in1=tcc[pb][rows, :], op0=ALU.mult, op1=ALU.add),
                              r=[tcc_d[pb], smd], w=[Cm_d[c]])
                    if A3_ST & 4:
                        if A3_E3 == "act":
                            kb.op("act", lambda e: e.activation(out=Cb[c][1 - par][rows, :], in_=Cm[c][rows, :], func=AF.Copy), r=[Cm_d[c]], w=[Cb_d[c][1 - par]])
                        else:
                            kb.op(A3_E3, lambda e: e.tensor_copy(out=Cb[c][1 - par][rows, :], in_=Cm[c][rows, :]), r=[Cm_d[c]], w=[Cb_d[c][1 - par]])
        finalize_batched(hsum, hs_d, O, vo_d, nw, 128, ps_o, ps_o_d)
        kb.pop()

    kb.pop()
    if "a4" in stages:
        kb.push()
        rdec = kb.sb([128, 4, 128], F32); rsc = kb.sb([128, 16], F32); rnw = kb.sb([128, 192], F32); rc_d = Dep()
        kb.dma("sp", rdec[:, :, :].rearrange("p a b -> p (a b)"), io["rdec"], wa=[rc_d])
        kb.dma("sp", rsc[:, :], io["rsc"], wa=[rc_d])
        kb.dma("sp", rnw[:, :], io["rnw"].partition_broadcast(128), wa=[rc_d])
        QrT = kb.sb([128, S], BF16); KrT = kb.sb([128, S], BF16); qrt_d = Dep()
        Kr = kb.sb([128, NCH, 128], BF16); kr_d = Dep()
        ps_o = kb.ps([128, 1024], BF16); ps_o_d = Dep()
        kb.push()
        Qr = kb.sb([128, NCH, 128], BF16); qr_d = Dep()
        Qs = kb.sb([128, NCH, 96], BF16); Ks = kb.sb([128, NCH, 96], BF16); qs_d = Dep()
        for q8 in range(8):
            kb.dma("sp", Qs[:, q8 * 8:(q8 + 1) * 8, :], tmv[:, q8 * 8:(q8 + 1) * 8, TM_RQ:TM_RQ + 96], r=[tm_d], wa=[qs_d])
            kb.dma("sp", Ks[:, q8 * 8:(q8 + 1) * 8, :], tmv[:, q8 * 8:(q8 + 1) * 8, TM_RK:TM_RK + 96], r=[tm_d], wa=[qs_d])
        rcos = kb.sb([128, NCH, 24], F32); rsin = kb.sb([128, NCH, 24], F32)
        kb.dma("sp", rcos[:, :, :].rearrange("p a b -> p (a b)"), io["rcos"], wa=[rc_d])
        kb.dma("sp", rsin[:, :, :].rearrange("p a b -> p (a b)"), io["rsin"], wa=[rc_d])
        t1 = kb.sb([128, NCH, 2, 24], F32); t2 = kb.sb([128, NCH, 2, 24], F32); t_d = Dep()
        kb.op("dve", lambda e: e.memset(Qr[:, :, :].rearrange("p a b -> p (a b)"), 0.0), w=[qr_d])
        kb.op("pool", lambda e: e.memset(Kr[:, :, :].rearrange("p a b -> p (a b)"), 0.0), w=[kr_d])
        kb.op("dve", lambda e: e.tensor_scalar(out=Ks[:, :, :].rearrange("p a b -> p (a b)"), in0=Ks[:, :, :].rearrange("p a b -> p (a b)"), scalar1=48.0 ** -0.5, scalar2=None, op0=ALU.mult), r=[qs_d], w=[qs_d])
        cos_b = rcos[:, :, :].unsqueeze(2).broadcast_to([128, NCH, 2, 24]); sin_b = rsin[:, :, :].unsqueeze(2).broadcast_to([128, NCH, 2, 24])
        for (Xs, Xr, xd) in ((Qs, Qr, qr_d), (Ks, Kr, kr_d)):
            X5 = Xs[:, :, :].rearrange("p n (h i two) -> p n h i two", h=2, two=2)
            xe = X5[:, :, :, :, 0]; xo = X5[:, :, :, :, 1]
            R5 = Xr[:, :, :].rearrange("p n (h r) -> p n h r", h=2)[:, :, :, 0:48].rearrange("p n h (i two) -> p n h i two", two=2)
            kb.op("dve", lambda e: e.tensor_tensor(out=t1[:, :, :, :], in0=xe, in1=cos_b, op=ALU.mult), r=[qs_d, rc_d], w=[t_d])
            kb.op("dve", lambda e: e.tensor_tensor(out=t2[:, :, :, :], in0=xo, in1=sin_b, op=ALU.mult), r=[qs_d, rc_d], wa=[t_d])
            kb.op("dve", lambda e: e.tensor_tensor(out=R5[:, :, :, :, 0], in0=t1[:, :, :, :], in1=t2[:, :, :, :], op=ALU.subtract), r=[t_d], w=[xd])
            kb.op("dve", lambda e: e.tensor_tensor(out=t1[:, :, :, :], in0=xe, in1=sin_b, op=ALU.mult), r=[qs_d, rc_d, xd], w=[t_d])
            kb.op("dve", lambda e: e.tensor_tensor(out=t2[:, :, :, :], in0=xo, in1=cos_b, op=ALU.mult), r=[qs_d, rc_d], wa=[t_d])
            kb.op("dve", lambda e: e.tensor_tensor(out=R5[:, :, :, :, 1], in0=t1[:, :, :, :], in1=t2[:, :, :, :], op=ALU.add), r=[t_d, xd], w=[xd])
        for (Xr, xd, XT) in ((Qr, qr_d, QrT), (Kr, kr_d, KrT)):
            for n8 in range(NCH // 8):
                for i8 in range(8):
                    n = n8 * 8 + i8
                    kb.op("pe", lambda e: e.transpose(ps_o[:, i8 * 128:(i8 + 1) * 128], Xr[:, n, :], ident_b[:, :]), r=[xd, cd],
                          w=[ps_o_d] if i8 == 0 else (), wa=() if i8 == 0 else [ps_o_d])
                kb.op("act", lambda e: e.activation(out=XT[:, n8 * 1024:(n8 + 1) * 1024], in_=ps_o[:, :], func=AF.Copy), r=[ps_o_d], wa=[qrt_d])
        kb.pop()
        V = kb.sb([128, NCH, 192], BF16); Gg = kb.sb([128, NCH, 192], BF16); vg_d = Dep(); gg_d = Dep()
        for q8 in range(8):
            kb.dma("sp", V[:, q8 * 8:(q8 + 1) * 8, :], tmv[:, q8 * 8:(q8 + 1) * 8, TM_RV:TM_RV + 192], r=[tm_d], wa=[vg_d])
            kb.dma("sp", Gg[:, q8 * 8:(q8 + 1) * 8, :], tmv[:, q8 * 8:(q8 + 1) * 8, TM_RG:TM_RG + 192], r=[tm_d], wa=[gg_d])
        ysum = kb.sb([128, NCH, 192], BF16); ys_d = [Dep() for _ in range(NCH)]
        sigmoid_inplace(Gg, gg_d, mul_by_self=True)
        Sm = [kb.sb([128, 96], F32) for _ in range(4)]; Sb = [[kb.sb([128, 96], BF16) for _ in range(2)] for _ in range(4)]
        Sm_d = [Dep() for _ in range(4)]; Sb_d = [[Dep() for _ in range(2)] for _ in range(4)]
        for c in range(4):
            kb.op("dve", lambda e: e.memset(Sm[c][:, :], 0.0), w=[Sm_d[c]])
            kb.op("dve", lambda e: e.memset(Sb[c][0][:, :], 0.0), w=[Sb_d[c][0]])
        ps_st = kb.ps([128, 512], F32); ps_st_d = Dep()
        ps_io = [kb.ps([128, 512], F32) for _ in range(2)]; ps_io_d = [Dep() for _ in range(2)]
        STd = [kb.sb([128, 128], BF16) for _ in range(2)]; STd_d = [Dep() for _ in range(2)]
        vp = [kb.sb([128, 96], BF16) for _ in range(2)]; vp_d = [Dep() for _ in range(2)]
        o1 = [kb.sb([128, 96], F32) for _ in range(2)]; o1_d = [Dep() for _ in range(2)]
        for d in range(2):
            order = range(NCH) if d == 0 else range(NCH - 1, -1, -1)
            for step, n in enumerate(order):
                ncol = slice(n * 128, (n + 1) * 128)
                for hl in range(2):
                    c = hl * 2 + d
                    b0 = 64 * hl
                    pb = hl
                    par = step % 2
                    rows = slice(b0, b0 + 48)
                    kb.op("pe", lambda e: e.matmul(ps_st[:, 0:128], KrT[b0:b0 + 48, ncol], QrT[b0:b0 + 48, ncol], start=True, stop=True), r=[qrt_d], w=[ps_st_d], drain=True)
                    kb.op("dve", lambda e: e.tensor_tensor(out=STd[pb][:, :], in0=ps_st[:, 0:128], in1=rdec[:, c, :], op=ALU.mult), r=[ps_st_d, rc_d], w=[STd_d[pb]])
                    kb.op("pool", lambda e: e.tensor_scalar(out=vp[pb][:, :], in0=V[:, n, hl * 96:(hl + 1) * 96], scalar1=rsc[:, c * 4:c * 4 + 1], scalar2=None, op0=ALU.mult),
                          r=[vg_d, rc_d], w=[vp_d[pb]])
                    P = ps_io[pb]
                    kb.op("pe", lambda e: e.matmul(P[:, 0:96], STd[pb][:, :], V[:, n, hl * 96:(hl + 1) * 96], start=True, stop=True), r=[STd_d[pb], vg_d], w=[ps_io_d[pb]])
                    kb.op("pe", lambda e: e.matmul(P[:, 128:224], QrT[b0:b0 + 48, ncol], Sb[c][par][b0:b0 + 48, :], start=True, stop=True), r=[qrt_d, Sb_d[c][par]], wa=[ps_io_d[pb]])
                    kb.op("pe", lambda e: e.matmul(P[0:b0 + 48, 256:352], Kr[:, n, 0:b0 + 48], vp[pb][:, :], start=True, stop=True), r=[kr_d, vp_d[pb]], wa=[ps_io_d[pb]])
                    kb.op("dve", lambda e: e.tensor_copy(out=o1[pb][:, :], in_=P[:, 0:96]), r=[ps_io_d[pb]], w=[o1_d[pb]])
                    ydst = ysum[:, n, hl * 96:(hl + 1) * 96]
                    if d == 0:
                        kb.op("dve", lambda e: e.scalar_tensor_tensor(out=ydst, in0=P[:, 128:224], scalar=rsc[:, c * 4 + 1:c * 4 + 2], in1=o1[pb][:, :], op0=ALU.mult, op1=ALU.add),
                              r=[ps_io_d[pb], o1_d[pb], rc_d], w=[ys_d[n]] if hl == 0 else (), wa=() if hl == 0 else [ys_d[n]])
                    else:
                        kb.op("dve", lambda e: e.scalar_tensor_tensor(out=o1[pb][:, :], in0=P[:, 128:224], scalar=rsc[:, c * 4 + 1:c * 4 + 2], in1=o1[pb][:, :], op0=ALU.mult, op1=ALU.add),
                              r=[ps_io_d[pb], rc_d], w=[o1_d[pb]])
                        kb.op("dve", lambda e: e.tensor_tensor(out=ydst, in0=ydst, in1=o1[pb][:, :], op=ALU.add), r=[o1_d[pb], ys_d[n]], w=[ys_d[n]])
                    kb.op("dve", lambda e: e.scalar_tensor_tensor(out=Sm[c][rows, :], in0=Sm[c][rows, :], scalar=rsc[rows, c * 4 + 2:c * 4 + 3], in1=P[rows, 256:352], op0=ALU.mult, op1=ALU.add),
                          r=[ps_io_d[pb]], w=[Sm_d[c]])
                    kb.op("pool", lambda e: e.tensor_copy(out=Sb[c][1 - par][rows, :], in_=Sm[c][rows, :]), r=[Sm_d[c]], w=[Sb_d[c][1 - par]])
        finalize_batched(ysum, ys_d, Gg, gg_d, rnw, 320, ps_o, ps_o_d)
        kb.pop()
    kb.barrier()


D = 1024
NE = 32
FF = 1024
ALPHA = (2.0 * 4) ** 0.25
EPS = 1e-5
SW_ALPHA = 1.702
SW_LIM = 7.0


def consts_b(C):
    c = {}
    c["ident_f"] = np.eye(128, dtype=np.float32)
    c["ustrict"] = np.triu(np.ones((128, 128), np.float32), 1)
    c["ones_f"] = np.ones((128, 128), np.float32)
    c["ec"] = np.tile((np.arange(NE, dtype=np.float32) * C)[None, :], (128, 1))
    return c


def layer_norm_tile(kb, r_t, r_d, g_t, b_t, out_t, out_d, scr, scr_d, eng2="pool", pd=()):
    st, mv, rstd = scr
    kb.op("dve", lambda e: e.bn_stats(out=st[:, 0, :], in_=r_t[:, 0:512]), r=[r_d], w=[scr_d])
    kb.op("dve", lambda e: e.bn_stats(out=st[:, 1, :], in_=r_t[:, 512:1024]), r=[r_d], wa=[scr_d])
    kb.op("dve", lambda e: e.bn_aggr(out=mv[:, :], in_=st[:, :, :].rearrange("p a b -> p (a b)")), r=[scr_d], wa=[scr_d])
    kb.op("act", lambda e: e.activation(out=rstd[:, :], in_=mv[:, 1:2], func=AF.Ln, bias=EPS), r=[scr_d], wa=[scr_d])
    kb.op("act", lambda e: e.activation(out=rstd[:, :], in_=rstd[:, :], func=AF.Exp, scale=-0.5), r=[scr_d], wa=[scr_d])
    kb.op("dve", lambda e: e.tensor_scalar(out=out_t, in0=r_t, scalar1=mv[:, 0:1], scalar2=rstd[:, 0:1],
                                           op0=ALU.subtract, op1=ALU.mult), r=[r_d, scr_d], w=[out_d])
    kb.op(eng2, lambda e: e.tensor_tensor(out=out_t, in0=out_t, in1=g_t, op=ALU.mult), r=[out_d] + list(pd), w=[out_d])
    kb.op(eng2, lambda e: e.tensor_tensor(out=out_t, in0=out_t, in1=b_t, op=ALU.add), r=[out_d] + list(pd), w=[out_d])


def build_b(nc, kb, io, NT, C, want_xT=True):
    TT = NT // 128
    NJ = C // 128
    TRASH = NE * C
    ident_f = kb.sb([128, 128], F32); ident_b = kb.sb([128, 128], BF16)
    ustrict_b = kb.sb([128, 128], BF16); ones_b = kb.sb([128, 128], BF16); ones_f = kb.sb([128, 128], F32)
    ec = kb.sb([128, NE], F32)
    cd = Dep()
    onesrow = kb.sb([1, 512], BF16)
    kb.op("dve", lambda e: e.memset(onesrow[:, :], 1.0), wa=[cd])
    kb.dma("sp", ident_f[:, :], io["ident_f"], w=[cd])
    kb.dma("sp", ones_f[:, :], io["ones_f"], wa=[cd])
    kb.dma("sp", ec[:, :], io["ec"], wa=[cd])
    kb.dma("pool", ident_b[:, :], io["ident_f"], wa=[cd])
    kb.dma("pool", ustrict_b[:, :], io["ustrict"], wa=[cd])
    kb.dma("pool", ones_b[:, :], io["ones_f"], wa=[cd])
    zrow = kb.sb([1, D], F32)
    kb.op("dve", lambda e: e.memset(zrow[:, :], 0.0), wa=[cd])
    ypad_d = Dep(); xg_d = Dep()
    kb.dma("sp", io["ypad"][TRASH:TRASH + 1, :], zrow[:, :], r=[cd], w=[ypad_d])
    kb.push()
    zt = kb.sb([128, 8 * D], BF16); zt_d = Dep()
    kb.op("pool", lambda e: e.memset(zt[:, :], 0.0), w=[zt_d])
    nrow = NE * C + 1
    r0 = 0
    while r0 < nrow:
        nr = min(1024, nrow - r0)
        if nr == 1024:
            kb.dma("sp", io["xg"][r0:r0 + 1024, :].rearrange("(p j) d -> p (j d)", p=128), zt[:, :], r=[zt_d], wa=[xg_d])
        else:
            kb.dma("sp", io["xg"][r0:r0 + nr, :], zt[0:nr, 0:D], r=[zt_d], wa=[xg_d])
        r0 += nr
    kb.pop()
    macc = kb.sb([128, NE], BF16); macc_d = Dep()
    kb.op("dve", lambda e: e.memset(macc[:, :], 0.0), w=[macc_d])
    idx = kb.sb([128, TT, 4], I32); gate = kb.sb([128, TT, 4], F32)
    idx_d = [Dep() for _ in range(TT)]

    NB = 2
    ps_mix = kb.ps([128, D], F32); ps_mix_d = Dep()
    ps_tr = kb.ps([128, D], F32); ps_tr_d = Dep()
    ps_misc = kb.ps([128, 2, 512], F32)
    ps_lg = ps_misc[:, 0, :]; ps_lg_d = Dep()
    ps_rk = ps_misc[:, 1, :]; ps_rk_d = Dep()
    ps_t = kb.ps([128, D], BF16); ps_t_d = Dep()
    kb.push()
    wout = kb.sb([128, 8, D], BF16)
    for c in range(8):
        kb.dma("pool", wout[:, c, :], io["w_out"][c * 128:(c + 1) * 128, :], wa=[cd])
    lnp = {}
    for nm in ("ln1_g", "ln1_b"):
        lnp[nm] = kb.sb([128, D], F32)
        kb.dma("sp", lnp[nm][:, :], io[nm].partition_broadcast(128), wa=[cd])
    wr = kb.sb([128, 8, NE], F32)
    kb.dma("sp", wr[:, :, :], io["w_router"].rearrange("(c p) e -> p c e", p=128), wa=[cd])
    br = kb.sb([1, NE], F32)
    kb.dma("sp", br[:, :], io["b_router"].rearrange("(o e) -> o e", o=1), wa=[cd])
    xr = [kb.sb([128, D], F32) for _ in range(NB)]; xr_d = [Dep() for _ in range(NB)]
    rt = [kb.sb([128, D], F32) for _ in range(NB)]; rt_d = [Dep() for _ in range(NB)]
    x1 = [kb.sb([128, D], F32) for _ in range(NB)]; x1_d = [Dep() for _ in range(NB)]
    scr = [(kb.sb([128, 2, 6], F32), kb.sb([128, 2], F32), kb.sb([128, 1], F32)) for _ in range(NB)]
    scr_d = [Dep() for _ in range(NB)]
    ymt = [kb.sb([128, 8, 128], BF16) for _ in range(NB)]; ymt_d = [Dep() for _ in range(NB)]
    x1b = [kb.sb([128, D], BF16) for _ in range(NB)]; x1b_d = [Dep() for _ in range(NB)]
    x1T = [kb.sb([128, 8, 128], F32) for _ in range(NB)]; x1T_d = [Dep() for _ in range(NB)]
    lg = [kb.sb([128, NE], F32) for _ in range(NB)]; lg_d = [Dep() for _ in range(NB)]
    m8 = [kb.sb([128, 8], F32) for _ in range(NB)]
    msk = [kb.sb([128, NE], BF16) for _ in range(NB)]; msk_d = [Dep() for _ in range(NB)]
    sm = [kb.sb([128, 64], F32) for _ in range(NB)]
    tmp32 = [kb.sb([128, NE], F32) for _ in range(NB)]
    destf = [kb.sb([128, NE], F32) for _ in range(NB)]
    valid = [kb.sb([128, NE], F32) for _ in range(NB)]

    for i in range(TT):
        b = i % NB
        tsl = slice(i * 128, (i + 1) * 128)
        kb.dma("sp", ymt[b][:, :, :], io["ymT"][:, tsl].rearrange("(c p) t -> p c t", p=128), w=[ymt_d[b]])
        kb.dma("sp", xr[b][:, :], io["xres"][tsl, :], w=[xr_d[b]])
        for h in range(2):
            for c in range(8):
                kb.op("pe", lambda e: e.matmul(ps_mix[:, h * 512:(h + 1) * 512], ymt[b][:, c, :],
                                               wout[:, c, h * 512:(h + 1) * 512], start=(c == 0), stop=(c == 7)),
                      r=[ymt_d[b], cd], w=[ps_mix_d] if (h == 0 and c == 0) else (), wa=() if (h == 0 and c == 0) else [ps_mix_d])
        kb.op("dve", lambda e: e.scalar_tensor_tensor(out=rt[b][:, :], in0=xr[b][:, :], scalar=ALPHA, in1=ps_mix[:, :],
                                                      op0=ALU.mult, op1=ALU.add), r=[xr_d[b], ps_mix_d], w=[rt_d[b]])
        layer_norm_tile(kb, rt[b][:, :], rt_d[b], lnp["ln1_g"][:, :], lnp["ln1_b"][:, :], x1[b][:, :], x1_d[b], scr[b], scr_d[b], pd=[cd])
        kb.dma("sp", io["x1s"][tsl, :], x1[b][:, :], r=[x1_d[b]])
        kb.op("act", lambda e: e.activation(out=x1b[b][:, :], in_=x1[b][:, :], func=AF.Copy), r=[x1_d[b]], w=[x1b_d[b]])
        for c in range(8):
            kb.op("pe", lambda e: e.transpose(ps_tr[:, c * 128:(c + 1) * 128], x1[b][:, c * 128:(c + 1) * 128], ident_f[:, :]),
                  r=[x1_d[b], cd], w=[ps_tr_d] if c == 0 else (), wa=() if c == 0 else [ps_tr_d])
        kb.op("act", lambda e: e.activation(out=x1T[b][:, :, :].rearrange("p c t -> p (c t)"), in_=ps_tr[:, :], func=AF.Copy),
              r=[ps_tr_d], w=[x1T_d[b]])
        for c in range(8):
            kb.op("pe", lambda e: e.matmul(ps_lg[:, 0:NE], x1T[b][:, c, :], wr[:, c, :], start=(c == 0), stop=False),
                  r=[x1T_d[b], cd], w=[ps_lg_d] if c == 0 else (), wa=() if c == 0 else [ps_lg_d])
        kb.op("pe", lambda e: e.matmul(ps_lg[:, 0:NE], ones_f[0:1, :], br[0:1, :], start=False, stop=True), r=[cd], wa=[ps_lg_d])
        kb.op("dve", lambda e: e.tensor_copy(out=lg[b][:, :], in_=ps_lg[:, 0:NE]), r=[ps_lg_d], w=[lg_d[b]])
        L = lg_d[b]
        kb.op("dve", lambda e: e.max(out=m8[b][:, :], in_=lg[b][:, :]), r=[L], w=[L])
        kb.op("dve", lambda e: e.tensor_scalar(out=msk[b][:, :], in0=lg[b][:, :], scalar1=m8[b][:, 3:4], scalar2=None,
                                               op0=ALU.is_ge), r=[L], w=[msk_d[b]])
        kb.op("dve", lambda e: e.tensor_scalar(out=sm[b][:, 0:1], in0=m8[b][:, 0:1], scalar1=-1.0, scalar2=None, op0=ALU.mult), r=[L], w=[L])
        kb.op("act", lambda e: e.activation(out=sm[b][:, 4:8], in_=m8[b][:, 0:4], func=AF.Exp, bias=sm[b][:, 0:1], scale=1.0), r=[L], w=[L])
        kb.op("dve", lambda e: e.reduce_sum(out=sm[b][:, 1:2], in_=sm[b][:, 4:8], axis=AX.X), r=[L], w=[L])
        kb.op("dve", lambda e: e.reciprocal(out=sm[b][:, 2:3], in_=sm[b][:, 1:2]), r=[L], w=[L])
        kb.op("dve", lambda e: e.tensor_scalar(out=sm[b][:, 8:12], in0=sm[b][:, 4:8], scalar1=sm[b][:, 2:3], scalar2=None, op0=ALU.mult), r=[L], w=[L])
        kb.op("pe", lambda e: e.matmul(ps_rk[:, 0:NE], ustrict_b[:, :], msk[b][:, :], start=True, stop=False), r=[msk_d[b], cd], w=[ps_rk_d])
        kb.op("pe", lambda e: e.matmul(ps_rk[:, 0:NE], ones_b[:, :], macc[:, :], start=False, stop=True), r=[macc_d, cd], wa=[ps_rk_d])
        kb.op("dve", lambda e: e.tensor_tensor(out=macc[:, :], in0=macc[:, :], in1=msk[b][:, :], op=ALU.add), r=[msk_d[b], ps_rk_d], w=[macc_d])
        kb.op("dve", lambda e: e.tensor_scalar(out=valid[b][:, :], in0=ps_rk[:, 0:NE], scalar1=float(C), scalar2=None, op0=ALU.is_lt), r=[ps_rk_d, L], w=[L])
        kb.op("dve", lambda e: e.scalar_tensor_tensor(out=destf[b][:, :], in0=ps_rk[:, 0:NE], scalar=-float(TRASH), in1=ec[:, :],
                                                      op0=ALU.add, op1=ALU.add), r=[ps_rk_d, L, cd], w=[L])
        kb.op("dve", lambda e: e.tensor_tensor(out=destf[b][:, :], in0=destf[b][:, :], in1=valid[b][:, :], op=ALU.mult), r=[L], w=[L])
        for k in range(4):
            kb.op("dve", lambda e: e.scalar_tensor_tensor(out=tmp32[b][:, :], in0=lg[b][:, :], scalar=m8[b][:, k:k + 1], in1=destf[b][:, :],
                                                          op0=ALU.is_equal, op1=ALU.mult), r=[L], w=[L])
            kb.op("dve", lambda e: e.reduce_sum(out=sm[b][:, 16 + k:17 + k], in_=tmp32[b][:, :], axis=AX.X), r=[L], w=[L])
            kb.op("dve", lambda e: e.scalar_tensor_tensor(out=tmp32[b][:, :], in0=lg[b][:, :], scalar=m8[b][:, k:k + 1], in1=valid[b][:, :],
                                                          op0=ALU.is_equal, op1=ALU.mult), r=[L], w=[L])
            kb.op("dve", lambda e: e.reduce_sum(out=sm[b][:, 20 + k:21 + k], in_=tmp32[b][:, :], axis=AX.X), r=[L], w=[L])
        kb.op("dve", lambda e: e.tensor_scalar(out=idx[:, i, :], in0=sm[b][:, 16:20], scalar1=float(TRASH), scalar2=None, op0=ALU.add), r=[L], w=[idx_d[i]])
        kb.op("dve", lambda e: e.tensor_tensor(out=gate[:, i, :], in0=sm[b][:, 8:12], in1=sm[b][:, 20:24], op=ALU.mult), r=[L], wa=[idx_d[i]])
        for k in range(4):
            kb.dma("pool", None, None, r=[x1b_d[b], idx_d[i]], wa=[xg_d],
                   fn=lambda e: e.indirect_dma_start(out=io["xg"], out_offset=bass.IndirectOffsetOnAxis(ap=idx[:, i, k:k + 1], axis=0),
                                                     in_=x1b[b][:, :], in_offset=None))

    kb.pop()
    kb.push()
    w1s = [kb.sb([128, 8, 2 * FF], BF16) for _ in range(2)]; w1_d = [Dep() for _ in range(2)]
    w2s = [kb.sb([128, 8, D], BF16) for _ in range(2)]; w2_d = [Dep() for _ in range(2)]
    b1r = [kb.sb([1, 2 * FF], BF16) for _ in range(2)]; b2r = [kb.sb([1, D], BF16) for _ in range(2)]
    xgs = kb.sb([128, NJ, D], BF16); xgs_d = Dep()
    xgT = kb.sb([128, 8, C], BF16); xgT_d = Dep()
    actT = kb.sb([128, 8, C], BF16); actT_d = [Dep() for _ in range(8)]
    tg = kb.sb([128, C], F32); tg_d = Dep()
    ts_ = kb.sb([128, C], F32); ts_d = Dep()
    tu = kb.sb([128, C], F32); tu_d = Dep()
    yo = [kb.sb([128, D], F32) for _ in range(2)]; yo_d = [Dep() for _ in range(2)]
    ps_g = ps_tr[:, :].rearrange("p (a b) -> p a b", a=2); ps_g_d = ps_tr_d
    ps_u = ps_misc; ps_u_d = ps_lg_d
    ps_y = ps_mix; ps_y_d = ps_mix_d
    nsp = [(0, min(C, 512))] + ([(512, C)] if C > 512 else [])

    def load_w(e_):
        bb = e_ % 2
        for c in range(8):
            kb.dma("pool", w1s[bb][:, c, :], io["w1"][e_, c * 128:(c + 1) * 128, :], w=[w1_d[bb]] if c == 0 else (), wa=() if c == 0 else [w1_d[bb]])
        kb.dma("pool", b1r[bb][:, :], io["b1"][e_:e_ + 1, :], wa=[w1_d[bb]])
        for c in range(8):
            kb.dma("pool", w2s[bb][:, c, :], io["w2"][e_, c * 128:(c + 1) * 128, :], w=[w2_d[bb]] if c == 0 else (), wa=() if c == 0 else [w2_d[bb]])
        kb.dma("pool", b2r[bb][:, :], io["b2"][e_:e_ + 1, :], wa=[w2_d[bb]])

    load_w(0)
    for ex in range(NE):
        bb = ex % 2
        if ex + 1 < NE:
            load_w(ex + 1)
        kb.dma("sp", xgs[:, :, :], io["xg"][ex * C:(ex + 1) * C, :].rearrange("(j p) d -> p j d", p=128), r=[xg_d], w=[xgs_d])
        for j in range(NJ):
            for c in range(8):
                kb.op("pe", lambda e: e.transpose(ps_t[:, c * 128:(c + 1) * 128], xgs[:, j, c * 128:(c + 1) * 128], ident_b[:, :]),
                      r=[xgs_d, cd], w=[ps_t_d] if c == 0 else (), wa=() if c == 0 else [ps_t_d])
            kb.op("act", lambda e: e.activation(out=xgT[:, :, j * 128:(j + 1) * 128], in_=ps_t[:, :].rearrange("p (c t) -> p c t", c=8), func=AF.Copy),
                  r=[ps_t_d], w=[xgT_d] if j == 0 else (), wa=() if j == 0 else [xgT_d])
        for m in range(8):
            for (pst, pd, col0) in ((ps_g, ps_g_d, m * 128), (ps_u, ps_u_d, FF + m * 128)):
                first = True
                for hi, (n0, n1) in enumerate(nsp):
                    kb.op("pe", lambda e: e.matmul(pst[:, hi, 0:n1 - n0], b1r[bb][0:1, col0:col0 + 128], onesrow[0:1, 0:n1 - n0], start=True, stop=False),
                          r=[w1_d[bb], cd], w=[pd] if first else (), wa=() if first else [pd])
                    first = False
                    for c in range(8):
                        kb.op("pe", lambda e: e.matmul(pst[:, hi, 0:n1 - n0], w1s[bb][:, c, col0:col0 + 128], xgT[:, c, n0:n1], start=False, stop=(c == 7)),
                              r=[w1_d[bb], xgT_d], wa=[pd])
            for hi, (n0, n1) in enumerate(nsp):
                w_ = n1 - n0
                kb.op("dve", lambda e: e.tensor_scalar(out=tg[:, n0:n1], in0=ps_g[:, hi, 0:w_], scalar1=SW_LIM, scalar2=None, op0=ALU.min),
                      r=[ps_g_d], w=[tg_d] if hi == 0 else (), wa=() if hi == 0 else [tg_d])
                kb.op("dve", lambda e: e.tensor_scalar(out=tu[:, n0:n1], in0=ps_u[:, hi, 0:w_], scalar1=SW_LIM, scalar2=-SW_LIM, op0=ALU.min, op1=ALU.max),
                      r=[ps_u_d], w=[tu_d] if hi == 0 else (), wa=() if hi == 0 else [tu_d])
            kb.op("act", lambda e: e.activation(out=ts_[:, :], in_=tg[:, :], func=AF.Sigmoid, scale=SW_ALPHA), r=[tg_d], w=[ts_d])
            kb.op("pool", lambda e: e.tensor_tensor(out=ts_[:, :], in0=ts_[:, :], in1=tg[:, :], op=ALU.mult), r=[tg_d, ts_d], w=[ts_d])
            kb.op("dve", lambda e: e.scalar_tensor_tensor(out=actT[:, m, :], in0=tu[:, :], scalar=1.0, in1=ts_[:, :], op0=ALU.add, op1=ALU.mult),
                  r=[tu_d, ts_d], w=[actT_d[m]])
        for j in range(NJ):
            yb = j % 2
            for h in range(2):
                kb.op("pe", lambda e: e.matmul(ps_y[:, h * 512:(h + 1) * 512], ones_b[0:1, :], b2r[bb][0:1, h * 512:(h + 1) * 512], start=True, stop=False),
                      r=[w2_d[bb], cd], w=[ps_y_d] if h == 0 else (), wa=() if h == 0 else [ps_y_d])
                for c in range(8):
                    kb.op("pe", lambda e: e.matmul(ps_y[:, h * 512:(h + 1) * 512], actT[:, c, j * 128:(j + 1) * 128], w2s[bb][:, c, h * 512:(h + 1) * 512],
                                                   start=False, stop=(c == 7)), r=[w2_d[bb], actT_d[c]], wa=[ps_y_d])
            kb.op("act", lambda e: e.activation(out=yo[yb][:, :], in_=ps_y[:, :], func=AF.Copy), r=[ps_y_d], w=[yo_d[yb]])
            r0 = ex * C + j * 128
            kb.dma("sp", io["ypad"][r0:r0 + 128, :], yo[yb][:, :], r=[yo_d[yb]], wa=[ypad_d])

    kb.pop()
    kb.push()
    lnp = {}
    for nm in ("ln2_g", "ln2_b"):
        lnp[nm] = kb.sb([128, D], F32)
        kb.dma("sp", lnp[nm][:, :], io[nm].partition_broadcast(128), wa=[cd])
    xr = [kb.sb([128, D], F32) for _ in range(NB)]; xr_d = [Dep() for _ in range(NB)]
    rt = [kb.sb([128, D], F32) for _ in range(NB)]; rt_d = [Dep() for _ in range(NB)]
    x1 = [kb.sb([128, D], F32) for _ in range(NB)]; x1_d = [Dep() for _ in range(NB)]
    scr = [(kb.sb([128, 2, 6], F32), kb.sb([128, 2], F32), kb.sb([128, 1], F32)) for _ in range(NB)]
    scr_d = [Dep() for _ in range(NB)]
    yk = [kb.sb([128, 4, D], F32) for _ in range(2)]; yk_d = [Dep() for _ in range(2)]
    x2b = [kb.sb([128, D], BF16) for _ in range(2)]; x2b_d = [Dep() for _ in range(2)]
    x2T = [kb.sb([128, 8, 128], BF16) for _ in range(2)]; x2T_d = [Dep() for _ in range(2)]
    for i in range(TT):
        b = i % 2
        tsl = slice(i * 128, (i + 1) * 128)
        for k in range(4):
            kb.dma("pool", None, None, r=[ypad_d, idx_d[i]], w=[yk_d[b]] if k == 0 else (), wa=() if k == 0 else [yk_d[b]],
                   fn=lambda e: e.indirect_dma_start(out=yk[b][:, k, :], out_offset=None, in_=io["ypad"],
                                                     in_offset=bass.IndirectOffsetOnAxis(ap=idx[:, i, k:k + 1], axis=0)))
        kb.dma("sp", xr[b][:, :], io["x1s"][tsl, :], w=[xr_d[b]])
        kb.op("dve", lambda e: e.tensor_scalar(out=rt[b][:, :], in0=xr[b][:, :], scalar1=ALPHA, scalar2=None, op0=ALU.mult), r=[xr_d[b]], w=[rt_d[b]])
        for k in range(4):
            kb.op("dve", lambda e: e.scalar_tensor_tensor(out=rt[b][:, :], in0=yk[b][:, k, :], scalar=gate[:, i, k:k + 1], in1=rt[b][:, :],
                                                          op0=ALU.mult, op1=ALU.add), r=[yk_d[b], idx_d[i]], w=[rt_d[b]])
        layer_norm_tile(kb, rt[b][:, :], rt_d[b], lnp["ln2_g"][:, :], lnp["ln2_b"][:, :], x1[b][:, :], x1_d[b], scr[b], scr_d[b], pd=[cd])
        kb.dma("sp", io["x2"][tsl, :], x1[b][:, :], r=[x1_d[b]])
        if want_xT:
            kb.op("act", lambda e: e.activation(out=x2b[b][:, :], in_=x1[b][:, :], func=AF.Copy), r=[x1_d[b]], w=[x2b_d[b]])
            for c in range(8):
                kb.op("pe", lambda e: e.transpose(ps_t[:, c * 128:(c + 1) * 128], x2b[b][:, c * 128:(c + 1) * 128], ident_b[:, :]),
                      r=[x2b_d[b], cd], w=[ps_t_d] if c == 0 else (), wa=() if c == 0 else [ps_t_d])
            kb.op("act", lambda e: e.activation(out=x2T[b][:, :, :].rearrange("p c t -> p (c t)"), in_=ps_t[:, :], func=AF.Copy), r=[ps_t_d], w=[x2T_d[b]])
            kb.dma("sp", io["x2T"][:, tsl].rearrange("(c p) t -> p c t", p=128), x2T[b][:, :, :], r=[x2T_d[b]])
    kb.pop()
    kb.barrier()

COL_F = 0; COL_MQK = 256; COL_MV = 640; COL_MO = 1024; COL_MG = 1408; COL_RQ = 1424; COL_RK = 1616; COL_RV = 1808; COL_RG = 2192


def prep_a(inp, l, j):
    w = inp["w_in"][l]; bs = inp["b_in"][l]
    hs = [2 * j, 2 * j + 1]
    cols = []
    for blk in range(4):
        cols += [COL_MG + blk * 4 + h for h in hs]
    cols += [COL_RQ + h * 48 + i for h in hs for i in range(48)]
    cols += [COL_RK + h * 48 + i for h in hs for i in range(48)]
    cols += [COL_MV + h * 96 + i for h in hs for i in range(96)]
    cols += [COL_MO + h * 96 + i for h in hs for i in range(96)]
    cols += [COL_RV + h * 96 + i for h in hs for i in range(96)]
    cols += [COL_RG + h * 96 + i for h in hs for i in range(96)]
    o = {}
    o["wtm"] = np.ascontiguousarray(w[:, cols]); o["btm"] = np.ascontiguousarray(bs[cols])
    fcols = [COL_F + g * 64 + i for g in hs for i in range(64)]
    o["wfT"] = np.ascontiguousarray(w[:, fcols].T); o["bf"] = np.ascontiguousarray(bs[fcols])
    cw = inp["conv_w"][l]
    for nm, base in (("q", COL_MQK), ("k", COL_MQK + 192)):
        W = np.zeros((1024, 128), np.float32); B = np.zeros((128,), np.float32); CW = np.zeros((128, 3), np.float32)
        for hl, h in enumerate(hs):
            W[:, hl * 64:hl * 64 + 48] = w[:, base + h * 48:base + (h + 1) * 48]
            B[hl * 64:hl * 64 + 48] = bs[base + h * 48:base + (h + 1) * 48]
            CW[hl * 64:hl * 64 + 48, :] = cw[:, base - COL_MQK + h * 48:base - COL_MQK + (h + 1) * 48].T
        o["w" + nm] = W; o["b" + nm] = B; o["conv" + nm] = CW
    o["mnw"] = np.ascontiguousarray(inp["ml_norm_w"][l][hs[0] * 96:(hs[1] + 1) * 96])
    o["rnw"] = np.ascontiguousarray(inp["ret_norm_w"][l][hs[0] * 96:(hs[1] + 1) * 96])
    return o


def ymix_cols(j):
    hs = [2 * j, 2 * j + 1]
    cols = [g * 64 + i for g in hs for i in range(64)]
    cols += [256 + h * 96 + i for h in hs for i in range(96)]
    cols += [640 + h * 96 + i for h in hs for i in range(96)]
    return cols


def build_p0(nc, kb, io, NT):
    TT = NT // 128
    ident_b = kb.sb([128, 128], BF16); cd = Dep()
    kb.dma("pool", ident_b[:, :], io["ident_f"], w=[cd])
    g_t = kb.sb([128, D], F32); b_t = kb.sb([128, D], F32)
    kb.dma("sp", g_t[:, :], io["emb_g"].partition_broadcast(128), wa=[cd])
    kb.dma("sp", b_t[:, :], io["emb_b"].partition_broadcast(128), wa=[cd])
    xr = [kb.sb([128, D], F32) for _ in range(2)]; xr_d = [Dep() for _ in range(2)]
    x1 = [kb.sb([128, D], F32) for _ in range(2)]; x1_d = [Dep() for _ in range(2)]
    scr = [(kb.sb([128, 2, 6], F32), kb.sb([128, 2], F32), kb.sb([128, 1], F32)) for _ in range(2)]; scr_d = [Dep() for _ in range(2)]
    x2b = [kb.sb([128, D], BF16) for _ in range(2)]; x2b_d = [Dep() for _ in range(2)]
    x2T = [kb.sb([128, 8, 128], BF16) for _ in range(2)]; x2T_d = [Dep() for _ in range(2)]
    ps_t = kb.ps([128, D], BF16); ps_t_d = Dep()
    for i in range(TT):
        b = i % 2
        tsl = slice(i * 128, (i + 1) * 128)
        kb.dma("sp", xr[b][:, :], io["x"][tsl, :], w=[xr_d[b]])
        layer_norm_tile(kb, xr[b][:, :], xr_d[b], g_t[:, :], b_t[:, :], x1[b][:, :], x1_d[b], scr[b], scr_d[b], pd=[cd])
        kb.dma("sp", io["x2"][tsl, :], x1[b][:, :], r=[x1_d[b]])
        kb.op("act", lambda e: e.activation(out=x2b[b][:, :], in_=x1[b][:, :], func=AF.Copy), r=[x1_d[b]], w=[x2b_d[b]])
        for c in range(8):
            kb.op("pe", lambda e: e.transpose(ps_t[:, c * 128:(c + 1) * 128], x2b[b][:, c * 128:(c + 1) * 128], ident_b[:, :]),
                  r=[x2b_d[b], cd], w=[ps_t_d] if c == 0 else (), wa=() if c == 0 else [ps_t_d])
        kb.op("act", lambda e: e.activation(out=x2T[b][:, :, :].rearrange("p c t -> p (c t)"), in_=ps_t[:, :], func=AF.Copy), r=[ps_t_d], w=[x2T_d[b]])
        kb.dma("sp", io["x2T"][:, tsl].rearrange("(c p) t -> p c t", p=128), x2T[b][:, :, :], r=[x2T_d[b]])
    kb.barrier()


NT_CORE = 4096
CAP = 768


def _mk(build_fn, ins, outs, scratch=()):
    nc = bass.Bass("TRN2", target_bir_lowering=False)
    kb = KB(nc)
    io = {}
    for name, shape, dt in ins:
        io[name] = nc.dram_tensor(name, list(shape), dt, kind="ExternalInput").ap()
    for name, shape, dt in outs:
        io[name] = nc.dram_tensor(name, list(shape), dt, kind="ExternalOutput").ap()
    for name, shape, dt in scratch:
        io[name] = nc.dram_tensor(name, list(shape), dt, kind="Internal").ap()
    build_fn(nc, kb, io)
    return nc


def kernel(**inp):
    import ml_dtypes
    from concourse.bass_utils import run_bass_kernel_spmd
    bf = ml_dtypes.bfloat16
    inp = {k: np.asarray(v) for k, v in inp.items()}
    x = inp["x"]
    NCORE = 8
    cores = list(range(NCORE))
    cid = lambda b, j: b * 2 + j
    ident = np.eye(128, dtype=np.float32)
    nc0 = _mk(lambda nc, kb, io: build_p0(nc, kb, io, NT_CORE),
              [("x", [NT_CORE, D], F32), ("emb_g", [D], F32), ("emb_b", [D], F32), ("ident_f", [128, 128], F32)],
              [("x2", [NT_CORE, D], F32), ("x2T", [D, NT_CORE], BF16)])
    in_maps = []
    for b in range(4):
        for j in range(2):
            in_maps.append({"x": np.ascontiguousarray(x[b, j * NT_CORE:(j + 1) * NT_CORE]), "emb_g": inp["emb_ln_g"], "emb_b": inp["emb_ln_b"], "ident_f": ident})
    res = run_bass_kernel_spmd(nc0, in_maps, core_ids=cores).results
    xres = [res[c]["x2"] for c in cores]
    xT = [res[c]["x2T"] for c in cores]
    cA = [consts_a(j) for j in range(2)]
    insA = [("xT", [D, S], BF16), ("wtm", [D, 968], F32), ("btm", [968], F32), ("wfT", [128, D], F32), ("bf", [128], F32),
            ("wq", [D, 128], F32), ("wk", [D, 128], F32), ("bq", [128], F32), ("bk", [128], F32), ("convq", [128, 3], F32), ("convk", [128, 3], F32),
            ("mnw", [192], F32), ("rnw", [192], F32)] + [(k, list(v.shape), F32) for k, v in cA[0][0].items()]
    ncA = _mk(lambda nc, kb, io: build_a(nc, kb, io, None), insA, [("ymT", [512, S], BF16)], [("tm", [S, TM_N], BF16)])
    cB = consts_b(CAP)
    insB = [("ymT", [D, NT_CORE], BF16), ("xres", [NT_CORE, D], F32), ("w_out", [D, D], F32)] + [(nm, [D], F32) for nm in ("ln1_g", "ln1_b", "ln2_g", "ln2_b")] + \
           [("w_router", [D, NE], F32), ("b_router", [NE], F32), ("w1", [NE, D, 2 * FF], F32), ("b1", [NE, 2 * FF], F32), ("w2", [NE, FF, D], F32), ("b2", [NE, D], F32)] + \
           [(k, list(v.shape), F32) for k, v in cB.items()]
    ncB = _mk(lambda nc, kb, io: build_b(nc, kb, io, NT_CORE, CAP), insB, [("x2", [NT_CORE, D], F32), ("x2T", [D, NT_CORE], BF16)],
              [("x1s", [NT_CORE, D], F32), ("xg", [NE * CAP + 1, D], BF16), ("ypad", [NE * CAP + 1, D], F32)])
    perm = ymix_cols(0) + ymix_cols(1)
    for l in range(4):
        pa_ = [prep_a(inp, l, j) for j in range(2)]
        in_maps = []
        for b in range(4):
            xTb = np.concatenate([xT[cid(b, 0)], xT[cid(b, 1)]], axis=1)
            for j in range(2):
                m = {"xT": xTb}
                m.update(pa_[j]); m.update(cA[j][0])
                in_maps.append(m)
        resA = run_bass_kernel_spmd(ncA, in_maps, core_ids=cores).results
        w_out_p = np.ascontiguousarray(inp["w_out"][l][perm, :])
        in_maps = []
        for b in range(4):
            for j in range(2):
                tsl = slice(j * NT_CORE, (j + 1) * NT_CORE)
                ym = np.concatenate([resA[cid(b, 0)]["ymT"][:, tsl], resA[cid(b, 1)]["ymT"][:, tsl]], axis=0)
                m = {"ymT": np.ascontiguousarray(ym), "xres": xres[cid(b, j)], "w_out": w_out_p}
                for nm in ("ln1_g", "ln1_b", "ln2_g", "ln2_b", "w_router", "b_router", "w1", "b1", "w2", "b2"):
                    m[nm] = inp[nm][l]
                m.update(cB)
                in_maps.append(m)
        resB = run_bass_kernel_spmd(ncB, in_maps, core_ids=cores).results
        xres = [resB[c]["x2"] for c in cores]
        xT = [resB[c]["x2T"] for c in cores]
    out = np.empty((4, S, D), np.float32)
    for b in range(4):
        for j in range(2):
            out[b, j * NT_CORE:(j + 1) * NT_CORE] = xres[cid(b, j)]
    return out
```

```python
import numpy as np
import concourse.bass as bass
import concourse.mybir as mybir

F32 = mybir.dt.float32
BF16 = mybir.dt.bfloat16
I32 = mybir.dt.int32
U32 = mybir.dt.uint32
AF = mybir.ActivationFunctionType
ALU = mybir.AluOpType
AX = mybir.AxisListType


class Dep:
    __slots__ = ("w", "r", "pw")

    def __init__(self):
        self.w = {}
        self.r = {}
        self.pw = {}


def _merge(dst, src):
    for k, v in src.items():
        if dst.get(k, 0) < v:
            dst[k] = v


def _nruns(ap):
    try:
        dims = [(int(st), int(n)) for st, n in ap.ap if int(n) > 1]
        tot = 1
        for st, n in dims:
            tot *= n
        run = 1
        for st, n in sorted(dims, key=lambda x: abs(x[0])):
            if st == run:
                run *= n
            else:
                break
        return tot // max(run, 1)
    except Exception:
        return 1


class KB:
    def __init__(self, nc, nds=16):
        self.nc = nc
        self.E = {"pe": nc.tensor, "act": nc.scalar, "dve": nc.vector, "pool": nc.gpsimd, "sp": nc.sync}
        self.epoch = 0
        self.csem = {e: nc.alloc_semaphore("c_" + e) for e in ("pe", "act", "dve", "pool")}
        self.ccnt = {e: 0 for e in self.csem}
        self.dsem = {q: [nc.alloc_semaphore("d_%s%d" % (q, i)) for i in range(nds)] for q in ("sp", "act", "pool")}
        self.dcnt = {q: [0] * nds for q in self.dsem}
        self.drr = {q: 0 for q in self.dsem}
        self.seen = {e: {} for e in self.E}
        self.ntile = 0
        self.ndesc = 0
        self.ndma = 0
        import contextlib
        self._ES = contextlib.ExitStack
        self.scopes = [self._ES()]

    def push(self):
        self.scopes.append(self._ES())

    def pop(self):
        self.barrier()
        self.scopes.pop().close()

    def sb(self, shape, dtype, name=None):
        self.ntile += 1
        return self.scopes[-1].enter_context(self.nc.sbuf_tensor(name or "t%d" % self.ntile, list(shape), dtype))

    def ps(self, shape, dtype=F32, name=None):
        self.ntile += 1
        return self.scopes[-1].enter_context(self.nc.psum_tensor(name or "p%d" % self.ntile, list(shape), dtype))

    def _sem(self, k):
        return self.csem[k[1]] if k[0] == "c" else self.dsem[k[1]][k[2]]

    def new_epoch(self):
        self.barrier()
        self.epoch += 1
        for e in ("pe", "act", "dve", "pool"):
            self.csem[e] = self.nc.alloc_semaphore("c%d_%s" % (self.epoch, e))
            self.ccnt[e] = 0
        for e in self.seen:
            for k in [k for k in self.seen[e] if k[0] == "c" and k[1] != "cc"]:
                del self.seen[e][k]

    def _need(self, e, toks):
        seen = self.seen[e]
        for k, v in toks.items():
            if k[0] == "c" and k[1] != "cc":
                if k[2] < self.epoch:
                    continue
                if e == "pe" and k[1] == "pe":
                    continue
            if seen.get(k, 0) >= v:
                continue
            seen[k] = v
            self.E[e].wait_ge(self._sem(k), v)

    def _deps(self, r, w, wa):
        toks = {}
        for d in r:
            _merge(toks, d.w)
        for d in w:
            _merge(toks, d.w)
            _merge(toks, d.r)
        for d in wa:
            _merge(toks, d.r)
            _merge(toks, d.pw)
        return toks

    def _mark(self, tok, r, w, wa):
        k, v = tok
        for d in r:
            if d.r.get(k, 0) < v:
                d.r[k] = v
        for d in w:
            pw = dict(d.w)
            _merge(pw, d.r)
            d.pw = pw
            d.w = {k: v}
            d.r = {}
        for d in wa:
            if d.w.get(k, 0) < v:
                d.w[k] = v

    def op(self, e, fn, r=(), w=(), wa=(), drain=False):
        if drain and self.ccnt[e]:
            k = ("c", e, self.epoch)
            if self.seen[e].get(k, 0) < self.ccnt[e]:
                self.seen[e][k] = self.ccnt[e]
                self.E[e].wait_ge(self.csem[e], self.ccnt[e])
        self._need(e, self._deps(r, w, wa))
        ins = fn(self.E[e])
        self.ccnt[e] += 1
        ins.then_inc(self.csem[e], 1)
        self._mark((("c", e, self.epoch), self.ccnt[e]), r, w, wa)
        return ins

    def dma(self, q, out, in_, r=(), w=(), wa=(), fn=None, **kw):
        j = self.drr[q]
        self.drr[q] = (j + 1) % len(self.dsem[q])
        toks = self._deps(r, w, wa)
        if self.dcnt[q][j]:
            _merge(toks, {("d", q, j): self.dcnt[q][j]})
        self._need(q, toks)
        if fn is None:
            self.ndesc += max(_nruns(out), _nruns(in_)); self.ndma += 1
            ins = self.E[q].dma_start(out=out, in_=in_, **kw)
        else:
            ins = fn(self.E[q])
        self.dcnt[q][j] += 16
        ins.then_inc(self.dsem[q][j], 16)
        self._mark((("d", q, j), self.dcnt[q][j]), r, w, wa)
        return ins

    def cc(self, fn, r=(), w=(), wa=()):
        if not hasattr(self, "ccsem"):
            self.ccsem = self.nc.alloc_semaphore("cc_sem")
            self.cccnt = 0
            self.csem["cc"] = self.ccsem
            self.ccnt["cc"] = 0
        toks = self._deps(r, w, wa)
        if self.ccnt["cc"]:
            _merge(toks, {("c", "cc"): self.ccnt["cc"]})
        self._need("pool", toks)
        ins = fn(self.E["pool"])
        self.ccnt["cc"] += 1
        ins.then_inc(self.ccsem, 1)
        self._mark((("c", "cc"), self.ccnt["cc"]), r, w, wa)
        return ins

    def all_tokens(self):
        toks = {}
        for e, c in self.ccnt.items():
            if c:
                toks[("c", e, self.epoch) if e != "cc" else ("c", "cc")] = c
        for q, lst in self.dcnt.items():
            for j, c in enumerate(lst):
                if c:
                    toks[("d", q, j)] = c
        return toks

    def barrier(self, engines=None):
        toks = self.all_tokens()
        for e in (engines or self.E):
            self._need(e, toks)

A3STOP = 99
A3_ND = 2
A3_NN = 64
A3_E3 = 'pool'
A3_FIN = 1
A3_ST = 7

S = 8192
NCH = 64
D = 1024
TM_Z = 0
TM_MG = 256
TM_RQ = 264
TM_RK = 360
TM_MV = 456
TM_MO = 648
TM_RV = 840
TM_RG = 1032
TM_N = 1224
TM_BANKS = ((0, 456), (456, 840), (840, 1224))
RET_G0 = 5.0
RET_BOFF = 0.5


def consts_a(j):
    c = {}
    c["ident_f"] = np.eye(128, dtype=np.float32)
    c["ones_f"] = np.ones((128, 128), np.float32)
    a = np.arange(64, dtype=np.float64)
    ang = 2 * np.pi * np.outer(a, a) / 64.0
    C64, S64 = np.cos(ang), np.sin(ang)
    T = np.zeros((128, 256), np.float64)
    for g in range(2):
        T[g * 64:(g + 1) * 64, g * 64:(g + 1) * 64] = C64
        T[g * 64:(g + 1) * 64, 128 + g * 64:128 + (g + 1) * 64] = -S64
    c["fT"] = T.astype(np.float32)
    c["tabA"] = np.concatenate([C64, -S64], 1).astype(np.float32)
    c["tabB"] = np.concatenate([S64, C64], 1).astype(np.float32)
    p = np.arange(128, dtype=np.float64)
    tw = 2 * np.pi * np.outer(p, a) / 8192.0
    sc = 1.0 / np.sqrt(8192.0 * 64.0)
    c["ct4"] = np.tile((np.cos(tw) * sc)[:, None, :], (1, 4, 1)).reshape(128, 256).astype(np.float32)
    c["st4"] = np.tile((np.sin(tw) * sc)[:, None, :], (1, 4, 1)).reshape(128, 256).astype(np.float32)
    a2 = 2 * np.pi * np.outer(p, p) / 128.0
    c["C128"] = np.cos(a2).astype(np.float32)
    c["S128"] = np.sin(a2).astype(np.float32)
    s_ = np.arange(128)[:, None]; t_ = np.arange(128)[None, :]
    c["maskF"] = (s_ <= t_).astype(np.float32)
    c["maskB"] = (s_ >= t_).astype(np.float32)
    dec = np.zeros((128, 4, 128), np.float64)
    rsc = np.zeros((128, 16), np.float64)
    gch = np.zeros((4,), np.float64)
    for hl in range(2):
        h = 2 * j + hl
        for d in range(2):
            lg = np.log1p(-2.0 ** (-(RET_G0 + (RET_BOFF if d else 0.0)) - h))
            if d == 0:
                m = np.where(s_ <= t_, np.exp(lg * np.maximum(t_ - s_, 0)), 0.0)
                wkey = np.exp(lg * (127 - np.arange(128)))
                wint = np.exp(lg * (np.arange(128) + 1.0))
            else:
                m = np.where(s_ > t_, np.exp(lg * np.maximum(s_ - t_, 0)), 0.0)
                wkey = np.exp(lg * np.arange(128))
                wint = np.exp(lg * (128.0 - np.arange(128)))
            dec[:, hl * 2 + d, :] = m
            rsc[:, (hl * 2 + d) * 4 + 0] = wkey
            rsc[:, (hl * 2 + d) * 4 + 1] = wint
            rsc[:, (hl * 2 + d) * 4 + 2] = np.exp(lg * 128.0)
            gch[hl * 2 + d] = np.exp(lg * 128.0)
    c["rdec"] = dec.reshape(128, 512).astype(np.float32)
    c["rsc"] = rsc.astype(np.float32)
    inv = 1.0 / (10000.0 ** (np.arange(0, 48, 2, dtype=np.float64) / 48.0))
    pos = (np.arange(64)[None, :] * 128 + np.arange(128)[:, None]).astype(np.float64)
    angr = pos[:, :, None] * inv[None, None, :]
    angr = (pos.astype(np.float32)[:, :, None] * inv.astype(np.float32)[None, None, :]).astype(np.float32).astype(np.float64)
    c["rcos"] = np.cos(angr).reshape(128, 64 * 24).astype(np.float32)
    c["rsin"] = np.sin(angr).reshape(128, 64 * 24).astype(np.float32)
    return c, [float(x) for x in gch]


def ymT_write(kb, io, row_lo, nrows, t0, ncols, src, r, wa):
    if not isinstance(io["ymT"], list):
        kb.dma("sp", io["ymT"][row_lo:row_lo + nrows, t0:t0 + ncols], src[0:nrows, 0:ncols], r=r, wa=wa)
        return
    a = row_lo
    while a < row_lo + nrows:
        k = a // 128
        e_ = min(row_lo + nrows, (k + 1) * 128)
        kb.dma("sp", io["ymT"][k][a - k * 128:e_ - k * 128, t0:t0 + ncols], src[a - row_lo:e_ - row_lo, 0:ncols], r=r, wa=wa)
        a = e_


def build_a(nc, kb, io, gch, stages=("a1", "a2", "a3", "a4")):
    ident_f = kb.sb([128, 128], F32); ident_b = kb.sb([128, 128], BF16)
    ones_f = kb.sb([128, 128], F32); ones_b = kb.sb([128, 128], BF16)
    cd = Dep()
    kb.dma("sp", ident_f[:, :], io["ident_f"], w=[cd])
    kb.dma("sp", ones_f[:, :], io["ones_f"], wa=[cd])
    kb.dma("pool", ident_b[:, :], io["ident_f"], wa=[cd])
    kb.dma("pool", ones_b[:, :], io["ones_f"], wa=[cd])
    kb.push()
    G = kb.sb([128, NCH, 8], F32); G_d = Dep()
    qT = kb.sb([128, S], BF16); kT = kb.sb([128, S], BF16); qk_d = Dep()
    tm_d = Dep()
    ym_d = Dep()

    if "a1" in stages:
        kb.push()
        wt = kb.sb([128, 8, TM_N], BF16); wt_d = Dep()
        for c in range(8):
            kb.dma("pool", wt[:, c, 256:TM_N], io["wtm"][c * 128:(c + 1) * 128, :], wa=[wt_d])
        brow = kb.sb([1, TM_N], BF16)
        kb.dma("pool", brow[:, 256:TM_N], io["btm"].rearrange("(o n) -> o n", o=1), wa=[wt_d])
        wq = kb.sb([128, 8, 128], BF16); wk = kb.sb([128, 8, 128], BF16)
        kb.dma("pool", wq[:, :, :], io["wq"].rearrange("(c p) m -> p c m", p=128), wa=[wt_d])
        kb.dma("pool", wk[:, :, :], io["wk"].rearrange("(c p) m -> p c m", p=128), wa=[wt_d])
        bqk = kb.sb([128, 2], F32)
        kb.dma("sp", bqk[:, 0:1], io["bq"].rearrange("(p o) -> p o", o=1), wa=[wt_d])
        kb.dma("sp", bqk[:, 1:2], io["bk"].rearrange("(p o) -> p o", o=1), wa=[wt_d])
        cw = kb.sb([128, 6], F32)
        kb.dma("sp", cw[:, 0:3], io["convq"], wa=[wt_d])
        kb.dma("sp", cw[:, 3:6], io["convk"], wa=[wt_d])
        wfT = kb.sb([128, D], F32); fT = kb.sb([128, 256], F32); bfc = kb.sb([128, 1], F32)
        kb.dma("sp", wfT[:, :], io["wfT"], wa=[wt_d])
        kb.dma("sp", fT[:, :], io["fT"], wa=[wt_d])
        kb.dma("sp", bfc[:, :], io["bf"].rearrange("(p o) -> p o", o=1), wa=[wt_d])
        ps_f = kb.ps([128, 512], F32); ps_f_d = Dep()
        for c in range(8):
            kb.op("pe", lambda e: e.matmul(ps_f[:, 0:256], wfT[:, c * 128:(c + 1) * 128], fT[:, :], start=True, stop=True), r=[wt_d], w=[ps_f_d])
            kb.op("act", lambda e: e.activation(out=wt[:, c, 0:256], in_=ps_f[:, 0:256], func=AF.Copy), r=[ps_f_d], wa=[wt_d])
        kb.op("pe", lambda e: e.matmul(ps_f[0:1, 0:256], bfc[:, 0:1], fT[:, :], start=True, stop=True), r=[wt_d], w=[ps_f_d])
        kb.op("act", lambda e: e.activation(out=brow[0:1, 0:256], in_=ps_f[0:1, 0:256], func=AF.Copy), r=[ps_f_d], wa=[wt_d])

        qpre = kb.sb([128, S + 2], BF16); kpre = kb.sb([128, S + 2], BF16); pre_d = Dep()
        for tl in (qpre, kpre):
            kb.op("dve", lambda e: e.memset(tl[:, 0:1], 0.0), wa=[pre_d])
            kb.op("dve", lambda e: e.memset(tl[:, S + 1:S + 2], 0.0), wa=[pre_d])
        xt = [kb.sb([128, 8, 512], BF16) for _ in range(2)]; xt_d = [Dep() for _ in range(2)]
        stg = [kb.sb([128, 4, TM_N], BF16) for _ in range(2)]; stg_d = [Dep() for _ in range(2)]
        ps_q = kb.ps([128, 512], F32); ps_q_d = Dep()
        ps_k = kb.ps([128, 512], F32); ps_k_d = Dep()
        ps_t = [kb.ps([128, 3, 512], F32) for _ in range(1)]; ps_t_d = [[Dep() for _ in range(3)] for _ in range(1)]
        for g in range(16):
            b = g % 2
            if "xTg" in io:
                half, col = divmod(g * 512, 4096)
                for k4 in range(4):
                    xsrc = io["xTg"][k4][half * 256:(half + 1) * 256, col:col + 512].rearrange("(c p) t -> p c t", p=128)
                    kb.dma("sp", xt[b][:, 2 * k4:2 * k4 + 2, :], xsrc, w=[xt_d[b]] if k4 == 0 else (), wa=() if k4 == 0 else [xt_d[b]])
            else:
                xsrc = io["xT"][:, g * 512:(g + 1) * 512].rearrange("(c p) t -> p c t", p=128)
                kb.dma("sp", xt[b][:, :, :], xsrc, w=[xt_d[b]])
            for (pst, pd, wgt, col, dst) in ((ps_q, ps_q_d, wq, 0, qpre), (ps_k, ps_k_d, wk, 1, kpre)):
                for c in range(8):
                    kb.op("pe", lambda e: e.matmul(pst[:, :], wgt[:, c, :], xt[b][:, c, :], start=(c == 0), stop=(c == 7)),
                          r=[xt_d[b], wt_d], w=[pd] if c == 0 else (), wa=() if c == 0 else [pd])
                kb.op("act", lambda e: e.activation(out=dst[:, 1 + g * 512:1 + (g + 1) * 512], in_=pst[:, :], func=AF.Identity, bias=bqk[:, col:col + 1]),
                      r=[pd, wt_d], wa=[pre_d])
            for sub in range(4):
                pb = 0
                for bi, (c0, c1) in enumerate(TM_BANKS):
                    pd = ps_t_d[pb][bi]
                    kb.op("pe", lambda e: e.matmul(ps_t[pb][:, bi, 0:c1 - c0], ones_b[0:1, :], brow[0:1, c0:c1], start=True, stop=False), r=[wt_d, cd], w=[pd])
                    for c in range(8):
                        kb.op("pe", lambda e: e.matmul(ps_t[pb][:, bi, 0:c1 - c0], xt[b][:, c, sub * 128:(sub + 1) * 128], wt[:, c, c0:c1],
                                                       start=False, stop=(c == 7)), r=[xt_d[b], wt_d], wa=[pd])
                    eng = "act" if bi != 1 else "dve"
                    if eng == "act":
                        kb.op("act", lambda e: e.activation(out=stg[b][:, sub, c0:c1], in_=ps_t[pb][:, bi, 0:c1 - c0], func=AF.Copy), r=[pd],
                              w=[stg_d[b]] if (sub == 0 and bi == 0) else (), wa=() if (sub == 0 and bi == 0) else [stg_d[b]])
                    else:
                        kb.op("dve", lambda e: e.tensor_copy(out=stg[b][:, sub, c0:c1], in_=ps_t[pb][:, bi, 0:c1 - c0]), r=[pd], wa=[stg_d[b]])
                    if bi == 0:
                        kb.op("dve", lambda e: e.tensor_copy(out=G[:, g * 4 + sub, :], in_=ps_t[pb][:, 0, TM_MG:TM_MG + 8]), r=[pd], wa=[G_d])
            kb.dma("sp", io["tm"][g * 512:(g + 1) * 512, :].rearrange("(s p) c -> p s c", p=128), stg[b][:, :, :], r=[stg_d[b]], wa=[tm_d])
        tmpc = kb.sb([128, S], F32); tmpc_d = Dep()
        for (src, dst, o) in ((qpre, qT, 0), (kpre, kT, 3)):
            kb.op("dve", lambda e: e.tensor_scalar(out=tmpc[:, :], in0=src[:, 0:S], scalar1=cw[:, o:o + 1], scalar2=None, op0=ALU.mult), r=[pre_d, wt_d], w=[tmpc_d])
            kb.op("dve", lambda e: e.scalar_tensor_tensor(out=tmpc[:, :], in0=src[:, 1:S + 1], scalar=cw[:, o + 1:o + 2], in1=tmpc[:, :], op0=ALU.mult, op1=ALU.add), r=[pre_d], w=[tmpc_d])
            kb.op("dve", lambda e: e.scalar_tensor_tensor(out=tmpc[:, :], in0=src[:, 2:S + 2], scalar=cw[:, o + 2:o + 3], in1=tmpc[:, :], op0=ALU.mult, op1=ALU.add), r=[pre_d], w=[tmpc_d])
            if o == 0:
                kb.op("act", lambda e: e.activation(out=dst[:, :], in_=tmpc[:, :], func=AF.Silu), r=[tmpc_d], wa=[qk_d])
            else:
                kb.op("act", lambda e: e.activation(out=tmpc[:, :], in_=tmpc[:, :], func=AF.Silu), r=[tmpc_d], w=[tmpc_d])
                kb.op("dve", lambda e: e.tensor_scalar(out=dst[:, :], in0=tmpc[:, :], scalar1=48.0 ** -0.5, scalar2=None, op0=ALU.mult), r=[tmpc_d], wa=[qk_d])
        kb.pop()

    if "a2" in stages:
        kb.push()
        ZT = kb.sb([64, 128, 256], BF16); zt_d = Dep()
        for q4 in range(4):
            kb.dma("sp", ZT[:, q4 * 32:(q4 + 1) * 32, :], io["tm"].rearrange("(t p) c -> t p c", p=128)[:, q4 * 32:(q4 + 1) * 32, 0:256], r=[tm_d], wa=[zt_d])
        tabA = kb.sb([64, 128], BF16); tabB = kb.sb([64, 128], BF16); C128 = kb.sb([128, 128], BF16); S128 = kb.sb([128, 128], BF16)
        ct4 = kb.sb([128, 256], F32); st4 = kb.sb([128, 256], F32); fc_d = Dep()
        kb.dma("pool", tabA[:, :], io["tabA"], wa=[fc_d]); kb.dma("pool", tabB[:, :], io["tabB"], wa=[fc_d])
        kb.dma("pool", C128[:, :], io["C128"], wa=[fc_d]); kb.dma("pool", S128[:, :], io["S128"], wa=[fc_d])
        kb.dma("sp", ct4[:, :], io["ct4"], wa=[fc_d]); kb.dma("sp", st4[:, :], io["st4"], wa=[fc_d])
        H = kb.sb([128, 2, 128, 64], BF16); H_d = Dep()
        yfT = kb.sb([128, S], BF16); yf_d = Dep()
        psG = [kb.ps([128, 4, 128], F32) for _ in range(2)]; psG_d = [Dep() for _ in range(2)]
        Gs = [kb.sb([128, 4, 128], F32) for _ in range(2)]; Gs_d = [Dep() for _ in range(2)]
        t1 = [kb.sb([128, 4, 64], F32) for _ in range(2)]; t2 = [kb.sb([128, 4, 64], F32) for _ in range(2)]
        t3 = [kb.sb([128, 4, 64], F32) for _ in range(2)]; t4 = [kb.sb([128, 4, 64], F32) for _ in range(2)]
        ta_d = [Dep() for _ in range(2)]; tb_d = [Dep() for _ in range(2)]
        ct3 = ct4[:, :].rearrange("p (c k) -> p c k", c=4); st3 = st4[:, :].rearrange("p (c k) -> p c k", c=4)
        for cg in range(32):
            b = cg % 2
            for ci in range(4):
                c = cg * 4 + ci
                kb.op("pe", lambda e: e.matmul(psG[b][:, ci, :], ZT[:, :, c], tabA[:, :], start=True, stop=False), r=[zt_d, fc_d],
                      w=[psG_d[b]] if ci == 0 else (), wa=() if ci == 0 else [psG_d[b]])
                kb.op("pe", lambda e: e.matmul(psG[b][:, ci, :], ZT[:, :, 128 + c], tabB[:, :], start=False, stop=True), r=[zt_d, fc_d], wa=[psG_d[b]])
            kb.op("act", lambda e: e.activation(out=Gs[b][:, :, :], in_=psG[b][:, :, :], func=AF.Copy), r=[psG_d[b]], w=[Gs_d[b]])
            Gr = Gs[b][:, :, 0:64]; Gi = Gs[b][:, :, 64:128]
            kb.op("dve", lambda e: e.tensor_tensor(out=t1[b][:, :, :], in0=Gr, in1=ct3, op=ALU.mult), r=[Gs_d[b], fc_d], w=[ta_d[b]])
            kb.op("dve", lambda e: e.tensor_tensor(out=t2[b][:, :, :], in0=Gi, in1=st3, op=ALU.mult), r=[Gs_d[b]], wa=[ta_d[b]])
            kb.op("dve", lambda e: e.tensor_tensor(out=H[:, 0, cg * 4:(cg + 1) * 4, :], in0=t1[b][:, :, :], in1=t2[b][:, :, :], op=ALU.add), r=[ta_d[b]], wa=[H_d])
            kb.op("dve", lambda e: e.tensor_tensor(out=t3[b][:, :, :], in0=Gi, in1=ct3, op=ALU.mult), r=[Gs_d[b], fc_d], w=[tb_d[b]])
            kb.op("dve", lambda e: e.tensor_tensor(out=t4[b][:, :, :], in0=Gr, in1=st3, op=ALU.mult), r=[Gs_d[b]], wa=[tb_d[b]])
            kb.op("dve", lambda e: e.tensor_tensor(out=H[:, 1, cg * 4:(cg + 1) * 4, :], in0=t3[b][:, :, :], in1=t4[b][:, :, :], op=ALU.subtract), r=[tb_d[b]], wa=[H_d])
        yf3 = yfT[:, :].rearrange("c (k2 k1) -> c k1 k2", k1=64)
        for kg in range(16):
            b = kg % 2
            for ki in range(4):
                k1 = kg * 4 + ki
                kb.op("pe", lambda e: e.matmul(psG[b][:, ki, :], H[:, 0, :, k1], C128[:, :], start=True, stop=False), r=[H_d, fc_d],
                      w=[psG_d[b]] if ki == 0 else (), wa=() if ki == 0 else [psG_d[b]])
                kb.op("pe", lambda e: e.matmul(psG[b][:, ki, :], H[:, 1, :, k1], S128[:, :], start=False, stop=True), r=[H_d, fc_d], wa=[psG_d[b]])
            kb.op("act", lambda e: e.activation(out=yf3[:, kg * 4:(kg + 1) * 4, :], in_=psG[b][:, :, :], func=AF.Copy), r=[psG_d[b]], wa=[yf_d])
        ymT_write(kb, io, 0, 128, 0, S, yfT, [yf_d], [ym_d])
        kb.pop()

    def head_norm_fin(src, src_d, nw, gate_sig, hn, out_bf, out_d):
        st, mv, rs, tmpn, hn_d = hn
        for hl in range(2):
            kb.op("dve", lambda e: e.bn_stats(out=st[:, hl, :], in_=src[:, hl * 96:(hl + 1) * 96]), r=[src_d], w=[hn_d] if hl == 0 else (), wa=() if hl == 0 else [hn_d])
        for hl in range(2):
            kb.op("dve", lambda e: e.bn_aggr(out=mv[:, hl * 2:hl * 2 + 2], in_=st[:, hl, :]), r=[hn_d], wa=[hn_d])
        kb.op("act", lambda e: e.activation(out=rs[:, 0:2], in_=mv[:, 1:4:2], func=AF.Ln, bias=1e-5), r=[hn_d], wa=[hn_d])
        kb.op("act", lambda e: e.activation(out=rs[:, 0:2], in_=rs[:, 0:2], func=AF.Exp, scale=-0.5), r=[hn_d], wa=[hn_d])
        for hl in range(2):
            kb.op("dve", lambda e: e.tensor_scalar(out=tmpn[:, hl * 96:(hl + 1) * 96], in0=src[:, hl * 96:(hl + 1) * 96], scalar1=mv[:, hl * 2:hl * 2 + 1],
                                                   scalar2=rs[:, hl:hl + 1], op0=ALU.subtract, op1=ALU.mult), r=[src_d, hn_d], wa=[hn_d])
        if gate_sig is None:
            kb.op("dve", lambda e: e.tensor_tensor(out=out_bf, in0=tmpn[:, :], in1=nw, op=ALU.mult), r=[hn_d], w=[out_d])
        else:
            gt, gt_d = gate_sig
            kb.op("dve", lambda e: e.tensor_tensor(out=tmpn[:, :], in0=tmpn[:, :], in1=nw, op=ALU.mult), r=[hn_d], wa=[hn_d])
            kb.op("dve", lambda e: e.tensor_tensor(out=out_bf, in0=tmpn[:, :], in1=gt, op=ALU.mult), r=[hn_d, gt_d], w=[out_d])

    def sigmoid_of(dst, src, rd, wd, n):
        kb.op("act", lambda e: e.activation(out=dst, in_=src, func=AF.Exp, scale=-1.0), r=rd, w=[wd])
        kb.op("dve", lambda e: e.tensor_scalar(out=dst, in0=dst, scalar1=1.0, scalar2=None, op0=ALU.add), r=[wd], w=[wd])
        kb.op("dve", lambda e: e.reciprocal(out=dst, in_=dst), r=[wd], w=[wd])

    def emit_out(ytok_bf, ytok_d, ymS, ymS_d, n, ps_o, ps_o_d):
        for hl in range(2):
            kb.op("pe", lambda e: e.transpose(ps_o[0:96, hl * 128:(hl + 1) * 128], ytok_bf[:, hl * 96:(hl + 1) * 96], ident_b[:, :]), r=[ytok_d, cd],
                  w=[ps_o_d] if hl == 0 else (), wa=() if hl == 0 else [ps_o_d])
        for hl in range(2):
            kb.op("act", lambda e: e.activation(out=ymS[hl][:, n * 128:(n + 1) * 128], in_=ps_o[0:96, hl * 128:(hl + 1) * 128], func=AF.Copy), r=[ps_o_d], wa=[ymS_d])


    def sigmoid_inplace(T, T_d, mul_by_self):
        scr = kb.sb([128, 16 * 192], F32); scr_d = Dep()
        for q in range(4):
            src = T[:, q * 16:(q + 1) * 16, :].rearrange("p n c -> p (n c)")
            kb.op("act", lambda e: e.activation(out=scr[:, :], in_=src, func=AF.Exp, scale=-1.0), r=[T_d], w=[scr_d])
            kb.op("dve", lambda e: e.tensor_scalar(out=scr[:, :], in0=scr[:, :], scalar1=1.0, scalar2=None, op0=ALU.add), r=[scr_d], w=[scr_d])
            kb.op("dve", lambda e: e.reciprocal(out=scr[:, :], in_=scr[:, :]), r=[scr_d], w=[scr_d])
            if mul_by_self:
                kb.op("dve", lambda e: e.tensor_tensor(out=src, in0=src, in1=scr[:, :], op=ALU.mult), r=[scr_d, T_d], w=[T_d])
            else:
                kb.op("dve", lambda e: e.tensor_copy(out=src, in_=scr[:, :]), r=[scr_d, T_d], w=[T_d])

    def finalize_batched(hsum, hs_d, GT, GT_d, nw, row0, ps_o, ps_o_d):
        pre_gate = (row0 == 128)
        HN = 16
        hg = kb.sb([128, HN, 192], F32); hg_d = Dep()
        sq = kb.sb([128, HN, 192], F32); sq_d = Dep()
        st1 = kb.sb([128, HN * 2], F32); st2 = kb.sb([128, HN * 2], F32); st_d = Dep()
        yt = kb.sb([128, HN, 192], BF16); yt_d = Dep()
        stg = [[kb.sb([96, 512], BF16) for _ in range(2)] for _ in range(2)]; stg_d = [[Dep() for _ in range(2)] for _ in range(2)]
        g3 = lambda t: t[:, :, :].rearrange("p n (h c) -> p (n h) c", h=2)
        f2_ = lambda t: t[:, :, :].rearrange("p n c -> p (n c)")
        for q in range(NCH // HN):
            nsl = slice(q * HN, (q + 1) * HN)
            hq = hsum[:, nsl, :].rearrange("p n c -> p (n c)")
            gq = GT[:, nsl, :].rearrange("p n c -> p (n c)")
            hdeps = [hs_d[n] for n in range(q * HN, (q + 1) * HN)]
            if pre_gate:
                kb.op("dve", lambda e: e.tensor_tensor(out=f2_(hg), in0=hq, in1=gq, op=ALU.mult), r=hdeps + [GT_d], w=[hg_d])
            else:
                kb.op("dve", lambda e: e.tensor_copy(out=f2_(hg), in_=hq), r=hdeps, w=[hg_d])
            kb.op("pool", lambda e: e.tensor_tensor(out=f2_(sq), in0=f2_(hg), in1=f2_(hg), op=ALU.mult), r=[hg_d], w=[sq_d])
            kb.op("dve", lambda e: e.reduce_sum(out=st1[:, :], in_=g3(hg), axis=AX.X), r=[hg_d], w=[st_d])
            kb.op("dve", lambda e: e.reduce_sum(out=st2[:, :], in_=g3(sq), axis=AX.X), r=[sq_d, st_d], w=[st_d])
            S_ = lambda fn, eng="dve": kb.op(eng, fn, r=[st_d], w=[st_d])
            S_(lambda e: e.tensor_scalar(out=st1[:, :], in0=st1[:, :], scalar1=1.0 / 96.0, scalar2=None, op0=ALU.mult))
            S_(lambda e: e.tensor_scalar(out=st2[:, :], in0=st2[:, :], scalar1=1.0 / 96.0, scalar2=None, op0=ALU.mult))
            S_(lambda e: e.tensor_tensor(out=sq[:, 0, 0:HN * 2], in0=st1[:, :], in1=st1[:, :], op=ALU.mult))
            kb.op("dve", lambda e: e.tensor_tensor(out=st2[:, :], in0=st2[:, :], in1=sq[:, 0, 0:HN * 2], op=ALU.subtract), r=[st_d, sq_d], w=[st_d])
            S_(lambda e: e.activation(out=st2[:, :], in_=st2[:, :], func=AF.Ln, bias=1e-5), "act")
            S_(lambda e: e.activation(out=st2[:, :], in_=st2[:, :], func=AF.Exp, scale=-0.5), "act")
            mean_b = st1[:, :].unsqueeze(2).broadcast_to([128, HN * 2, 96])
            rstd_b = st2[:, :].unsqueeze(2).broadcast_to([128, HN * 2, 96])
            nw_b = nw[:, :].rearrange("p (h c) -> p h c", h=2).unsqueeze(1).broadcast_to([128, HN, 2, 96])
            kb.op("dve", lambda e: e.tensor_tensor(out=g3(hg), in0=g3(hg), in1=mean_b, op=ALU.subtract), r=[hg_d, st_d], w=[hg_d])
            kb.op("dve", lambda e: e.tensor_tensor(out=g3(hg), in0=g3(hg), in1=rstd_b, op=ALU.mult), r=[hg_d, st_d], w=[hg_d])
            hg4 = hg[:, :, :].rearrange("p n (h c) -> p n h c", h=2)
            if pre_gate:
                kb.op("dve", lambda e: e.tensor_tensor(out=yt[:, :, :].rearrange("p n (h c) -> p n h c", h=2), in0=hg4, in1=nw_b, op=ALU.mult), r=[hg_d], w=[yt_d])
            else:
                kb.op("dve", lambda e: e.tensor_tensor(out=hg4, in0=hg4, in1=nw_b, op=ALU.mult), r=[hg_d], w=[hg_d])
                kb.op("dve", lambda e: e.tensor_tensor(out=f2_(yt), in0=f2_(hg), in1=gq, op=ALU.mult), r=[hg_d, GT_d], w=[yt_d])
            for n4 in range(HN // 4):
                sb_ = (q * (HN // 4) + n4) % 2
                for i4 in range(4):
                    for hl in range(2):
                        kb.op("pe", lambda e: e.transpose(ps_o[0:96, (hl * 4 + i4) * 128:(hl * 4 + i4 + 1) * 128], yt[:, n4 * 4 + i4, hl * 96:(hl + 1) * 96], ident_b[:, :]),
                              r=[yt_d, cd], w=[ps_o_d] if (i4 == 0 and hl == 0) else (), wa=() if (i4 == 0 and hl == 0) else [ps_o_d])
                for hl in range(2):
                    kb.op("act", lambda e: e.activation(out=stg[hl][sb_][:, :], in_=ps_o[0:96, hl * 512:(hl + 1) * 512], func=AF.Copy), r=[ps_o_d], w=[stg_d[hl][sb_]])
                    t0_ = (q * HN + n4 * 4) * 128
                    ymT_write(kb, io, row0 + hl * 96, 96, t0_, 512, stg[hl][sb_], [stg_d[hl][sb_]], [ym_d])

    tmv = io["tm"].rearrange("(n p) c -> p n c", p=128)

    if "a3" in stages:
        kb.push()
        maskF = kb.sb([128, 128], F32); maskB = kb.sb([128, 128], F32); mk_d = Dep()
        kb.dma("sp", maskF[:, :], io["maskF"], wa=[mk_d]); kb.dma("sp", maskB[:, :], io["maskB"], wa=[mk_d])
        nw = kb.sb([128, 192], F32)
        kb.dma("sp", nw[:, :], io["mnw"].partition_broadcast(128), wa=[mk_d])
        V = kb.sb([128, NCH, 2, 98], BF16); O = kb.sb([128, NCH, 192], BF16); vo_d = Dep()
        kb.op("dve", lambda e: e.memset(V[:, :, :, 96:97], 1.0), wa=[vo_d])
        kb.op("dve", lambda e: e.memset(V[:, :, :, 97:98], 0.0), wa=[vo_d])
        for q8 in range(8):
            for hl in range(2):
                kb.dma("sp", V[:, q8 * 8:(q8 + 1) * 8, hl, 0:96], tmv[:, q8 * 8:(q8 + 1) * 8, TM_MV + hl * 96:TM_MV + (hl + 1) * 96], r=[tm_d], wa=[vo_d])
            kb.dma("sp", O[:, q8 * 8:(q8 + 1) * 8, :], tmv[:, q8 * 8:(q8 + 1) * 8, TM_MO:TM_MO + 192], r=[tm_d], wa=[vo_d])
        sm = {nm: kb.sb([128, NCH, 4], F32) for nm in ("Fp", "Ip", "lf", "a", "Aend", "b", "BM", "mch", "mnew", "mprev", "sprev", "scur", "M", "eM", "p", "fa", "fi", "t0")}
        smd = Dep()
        f2 = lambda nm: sm[nm][:, :, :].rearrange("p n c -> p (n c)")
        for d in range(2):
            kb.op("dve", lambda e: e.tensor_copy(out=sm["Ip"][:, :, d * 2:d * 2 + 2], in_=G[:, :, d * 4:d * 4 + 2]), r=[G_d], wa=[smd])
            kb.op("dve", lambda e: e.tensor_copy(out=sm["Fp"][:, :, d * 2:d * 2 + 2], in_=G[:, :, d * 4 + 2:d * 4 + 4]), r=[G_d], wa=[smd])
        S1 = lambda fn, eng="dve": kb.op(eng, fn, r=[smd], w=[smd])
        S1(lambda e: e.activation(out=f2("lf"), in_=f2("Fp"), func=AF.Exp, scale=-1.0), "act")
        S1(lambda e: e.activation(out=f2("lf"), in_=f2("lf"), func=AF.Ln, bias=1.0), "act")
        S1(lambda e: e.tensor_scalar(out=f2("lf"), in0=f2("lf"), scalar1=-1.0, scalar2=None, op0=ALU.mult))
        if A3STOP == 10:
            kb.pop(); kb.barrier(); return
        ps_s = kb.ps([128, 2, 512], F32); ps_s_d = Dep()
        for d in range(2):
            kb.op("pe", lambda e: e.matmul(ps_s[:, 0, d * 128:(d + 1) * 128], (maskF if d == 0 else maskB)[:, :], sm["lf"][:, :, d * 2:d * 2 + 2], start=True, stop=True),
                  r=[smd, mk_d], w=[ps_s_d] if d == 0 else (), wa=() if d == 0 else [ps_s_d])
        kb.op("pe", lambda e: e.matmul(ps_s[:, 1, 0:256], ones_f[:, :], f2("lf"), start=True, stop=True), r=[smd, cd], wa=[ps_s_d])
        for d in range(2):
            kb.op("dve", lambda e: e.tensor_copy(out=sm["a"][:, :, d * 2:d * 2 + 2], in_=ps_s[:, 0, d * 128:(d + 1) * 128].rearrange("p (n h) -> p n h", h=2)), r=[ps_s_d, smd], w=[smd])
        kb.op("dve", lambda e: e.tensor_copy(out=f2("Aend"), in_=ps_s[:, 1, 0:256]), r=[ps_s_d, smd], w=[smd])
        S1(lambda e: e.tensor_tensor(out=f2("b"), in0=f2("Ip"), in1=f2("a"), op=ALU.subtract))
        if A3STOP == 11:
            kb.pop(); kb.barrier(); return
        bmc = kb.sb([128, 2], F32); dg = kb.sb([128, 128], F32)
        for hf in range(2):
            kb.op("pe", lambda e: e.transpose(ps_s[:, 0, 0:128], f2("b")[:, hf * 128:(hf + 1) * 128], ident_f[:, :]), r=[smd, cd], w=[ps_s_d])
            kb.op("dve", lambda e: e.reduce_max(out=bmc[:, hf:hf + 1], in_=ps_s[:, 0, 0:128], axis=AX.X), r=[ps_s_d, smd], w=[smd])
            kb.op("dve", lambda e: e.tensor_scalar(out=dg[:, :], in0=ident_f[:, :], scalar1=bmc[:, hf:hf + 1], scalar2=None, op0=ALU.mult), r=[smd, cd], w=[smd])
            kb.op("pe", lambda e: e.matmul(ps_s[:, 1, 0:128], ones_f[:, :], dg[:, :], start=True, stop=True), r=[smd, cd], w=[ps_s_d])
            kb.op("dve", lambda e: e.tensor_copy(out=f2("BM")[:, hf * 128:(hf + 1) * 128], in_=ps_s[:, 1, 0:128]), r=[ps_s_d, smd], w=[smd])
        if A3STOP == 12:
            kb.pop(); kb.barrier(); return
        S1(lambda e: e.tensor_tensor(out=f2("mch"), in0=f2("Aend"), in1=f2("BM"), op=ALU.add))
        for c in range(4):
            sl = (lambda nm: sm[nm][:, :, c]) if c < 2 else (lambda nm: sm[nm][:, ::-1, c])
            S1(lambda e: e.tensor_tensor_scan(out=sl("mnew"), data0=sl("Aend"), data1=sl("mch"), initial=0.0, op0=ALU.add, op1=ALU.max))
        if A3STOP == 13:
            kb.pop(); kb.barrier(); return
        S1(lambda e: e.memset(f2("mprev"), 0.0))
        S1(lambda e: e.tensor_copy(out=sm["mprev"][:, 1:NCH, 0:2], in_=sm["mnew"][:, 0:NCH - 1, 0:2]))
        S1(lambda e: e.tensor_copy(out=sm["mprev"][:, 0:NCH - 1, 2:4], in_=sm["mnew"][:, 1:NCH, 2:4]))
        if A3STOP == 14:
            kb.pop(); kb.barrier(); return
        S1(lambda e: e.tensor_tensor(out=f2("t0"), in0=f2("Aend"), in1=f2("mprev"), op=ALU.add))
        S1(lambda e: e.tensor_tensor(out=f2("sprev"), in0=f2("t0"), in1=f2("mnew"), op=ALU.subtract))
        S1(lambda e: e.activation(out=f2("sprev"), in_=f2("sprev"), func=AF.Exp), "act")
        S1(lambda e: e.tensor_tensor(out=f2("scur"), in0=f2("mch"), in1=f2("mnew"), op=ALU.subtract))
        S1(lambda e: e.activation(out=f2("scur"), in_=f2("scur"), func=AF.Exp), "act")
        if A3STOP == 15:
            kb.pop(); kb.barrier(); return
        S1(lambda e: e.tensor_tensor(out=f2("M"), in0=f2("mprev"), in1=f2("BM"), op=ALU.max))
        S1(lambda e: e.activation(out=f2("eM"), in_=f2("M"), func=AF.Exp, scale=-1.0), "act")
        S1(lambda e: e.tensor_tensor(out=f2("p"), in0=f2("b"), in1=f2("BM"), op=ALU.subtract))
        S1(lambda e: e.activation(out=f2("p"), in_=f2("p"), func=AF.Exp), "act")
        S1(lambda e: e.tensor_tensor(out=f2("t0"), in0=f2("a"), in1=f2("M"), op=ALU.subtract))
        S1(lambda e: e.tensor_tensor(out=f2("fa"), in0=f2("t0"), in1=f2("BM"), op=ALU.add))
        S1(lambda e: e.activation(out=f2("fa"), in_=f2("fa"), func=AF.Exp), "act")
        S1(lambda e: e.tensor_tensor(out=f2("fi"), in0=f2("t0"), in1=f2("mprev"), op=ALU.add))
        S1(lambda e: e.activation(out=f2("fi"), in_=f2("fi"), func=AF.Exp), "act")
        if A3STOP == 1:
            kb.pop(); kb.barrier(); return
        ktok = kb.sb([128, NCH, 128], BF16); ktok_d = Dep()
        ps_o = kb.ps([128, 1024], BF16); ps_o_d = Dep()
        for n4 in range(NCH // 4):
            for q4 in range(4):
                n = n4 * 4 + q4
                kb.op("pe", lambda e: e.transpose(ps_o[:, q4 * 128:(q4 + 1) * 128], kT[:, n * 128:(n + 1) * 128], ident_b[:, :]), r=[qk_d, cd],
                      w=[ps_o_d] if q4 == 0 else (), wa=() if q4 == 0 else [ps_o_d])
            kb.op("act", lambda e: e.activation(out=ktok[:, n4 * 4:(n4 + 1) * 4, :].rearrange("p n c -> p (n c)"), in_=ps_o[:, 0:512], func=AF.Copy), r=[ps_o_d], wa=[ktok_d])
        if A3STOP == 2:
            kb.pop(); kb.barrier(); return
        hsum = kb.sb([128, NCH, 192], BF16); hs_d = [Dep() for _ in range(NCH)]
        sigmoid_inplace(O, vo_d, mul_by_self=False)
        Cm = [kb.sb([128, 98], F32) for _ in range(4)]; Cb = [[kb.sb([128, 98], BF16) for _ in range(2)] for _ in range(4)]
        Cm_d = [Dep() for _ in range(4)]; Cb_d = [[Dep() for _ in range(2)] for _ in range(4)]
        for c in range(4):
            kb.op("dve", lambda e: e.memset(Cm[c][:, :], 0.0), w=[Cm_d[c]])
            kb.op("dve", lambda e: e.memset(Cb[c][0][:, :], 0.0), w=[Cb_d[c][0]])
        ps_st = kb.ps([128, 512], F32); ps_st_d = Dep()
        ps_io = [kb.ps([128, 512], F32) for _ in range(2)]; ps_io_d = [Dep() for _ in range(2)]
        STd = [kb.sb([128, 128], BF16) for _ in range(2)]; STd_d = [Dep() for _ in range(2)]
        vaug = [kb.sb([128, 98], BF16) for _ in range(2)]; vaug_d = [Dep() for _ in range(2)]
        for t_ in vaug:
            kb.op("dve", lambda e: e.memset(t_[:, :], 0.0), w=[vaug_d[vaug.index(t_)]])
        o1 = [kb.sb([128, 98], F32) for _ in range(2)]; o1_d = [Dep() for _ in range(2)]
        dn = [kb.sb([128, 2], F32) for _ in range(2)]
        tcc = [kb.sb([128, 98], F32) for _ in range(2)]; tcc_d = [Dep() for _ in range(2)]
        sgo = kb.sb([128, 192], F32); sgo_d = Dep()
        hfin = kb.sb([128, 192], F32); hfin_d = Dep()
        ytok = [kb.sb([128, 192], BF16) for _ in range(2)]; ytok_d = [Dep() for _ in range(2)]
        hn = (kb.sb([128, 2, 6], F32), kb.sb([128, 4], F32), kb.sb([128, 2], F32), kb.sb([128, 192], F32), Dep())
        it = 0
        for d in range(A3_ND):
            order = range(NCH) if d == 0 else range(NCH - 1, -1, -1)
            msk = maskF if d == 0 else maskB
            for step, n in enumerate(list(order)[:A3_NN]):
                ncol = slice(n * 128, (n + 1) * 128)
                for hl in range(2):
                    c = d * 2 + hl
                    b0 = 64 * hl
                    pb = it % 2; it += 1
                    par = step % 2
                    kb.op("pe", lambda e: e.matmul(ps_st[:, 0:128], kT[b0:b0 + 48, ncol], qT[b0:b0 + 48, ncol], start=True, stop=True), r=[qk_d], w=[ps_st_d], drain=True)
                    kb.op("dve", lambda e: e.tensor_tensor(out=STd[pb][:, :], in0=ps_st[:, 0:128], in1=msk[:, :], op=ALU.mult), r=[ps_st_d, mk_d], w=[STd_d[pb]])
                    kb.op("dve", lambda e: e.tensor_scalar(out=vaug[pb][:, :], in0=V[:, n, hl, :], scalar1=sm["p"][:, n, c:c + 1], scalar2=None, op0=ALU.mult),
                          r=[vo_d, smd], w=[vaug_d[pb]])
                    P = ps_io[pb]
                    kb.op("pe", lambda e: e.matmul(P[:, 0:98], STd[pb][:, :], vaug[pb][:, :], start=True, stop=True), r=[STd_d[pb], vaug_d[pb]], w=[ps_io_d[pb]])
                    kb.op("pe", lambda e: e.matmul(P[:, 128:226], qT[b0:b0 + 48, ncol], Cb[c][par][b0:b0 + 48, :], start=True, stop=True), r=[qk_d, Cb_d[c][par]], wa=[ps_io_d[pb]])
                    kb.op("pe", lambda e: e.matmul(P[0:b0 + 48, 256:354], ktok[:, n, 0:b0 + 48], vaug[pb][:, :], start=True, stop=True), r=[ktok_d, vaug_d[pb]], wa=[ps_io_d[pb]])
                    kb.op("dve", lambda e: e.tensor_scalar(out=o1[pb][:, :], in0=P[:, 0:98], scalar1=sm["fa"][:, n, c:c + 1], scalar2=None, op0=ALU.mult), r=[ps_io_d[pb], smd], w=[o1_d[pb]])
                    kb.op("dve", lambda e: e.scalar_tensor_tensor(out=o1[pb][:, :], in0=P[:, 128:226], scalar=sm["fi"][:, n, c:c + 1], in1=o1[pb][:, :], op0=ALU.mult, op1=ALU.add),
                          r=[ps_io_d[pb], smd], w=[o1_d[pb]])
                    kb.op("dve", lambda e: e.scalar_tensor_tensor(out=dn[pb][:, 0:1], in0=o1[pb][:, 96:97], scalar=-1.0, in1=o1[pb][:, 96:97], op0=ALU.mult, op1=ALU.max), r=[o1_d[pb]], w=[o1_d[pb]])
                    kb.op("dve", lambda e: e.tensor_tensor(out=dn[pb][:, 0:1], in0=dn[pb][:, 0:1], in1=sm["eM"][:, n, c:c + 1], op=ALU.max), r=[o1_d[pb], smd], w=[o1_d[pb]])
                    kb.op("dve", lambda e: e.reciprocal(out=dn[pb][:, 1:2], in_=dn[pb][:, 0:1]), r=[o1_d[pb]], w=[o1_d[pb]])
                    hdst = hsum[:, n, hl * 96:(hl + 1) * 96]
                    if d == 0:
                        kb.op("dve", lambda e: e.tensor_scalar(out=hdst, in0=o1[pb][:, 0:96], scalar1=dn[pb][:, 1:2], scalar2=None, op0=ALU.mult), r=[o1_d[pb]],
                              w=[hs_d[n]] if hl == 0 else (), wa=() if hl == 0 else [hs_d[n]])
                    else:
                        kb.op("dve", lambda e: e.scalar_tensor_tensor(out=hdst, in0=o1[pb][:, 0:96], scalar=dn[pb][:, 1:2], in1=hdst, op0=ALU.mult, op1=ALU.add), r=[o1_d[pb], hs_d[n]], w=[hs_d[n]])
                    rows = slice(b0, b0 + 48)
                    kb.op("dve", lambda e: e.tensor_scalar(out=tcc[pb][rows, :], in0=P[rows, 256:354], scalar1=sm["scur"][rows, n, c:c + 1], scalar2=None, op0=ALU.mult), r=[ps_io_d[pb], smd], w=[tcc_d[pb]])
                    kb.op("dve", lambda e: e.scalar_tensor_tensor(out=Cb[c][1 - par][rows, :], in0=Cb[c][par][rows, :], scalar=sm["sprev"][rows, n, c:c + 1], in1=tcc[pb][rows, :], op0=ALU.mult, op1=ALU.add),
                          r=[tcc_d[pb], smd, Cb_d[c][par]], w=[Cb_d[c][1 - par]])
        finalize_batched(hsum, hs_d, O, vo_d, nw, 128, ps_o, ps_o_d)
        kb.pop()

    kb.pop()
    if "a4" in stages:
        kb.push()
        rdec = kb.sb([128, 4, 128], F32); rsc = kb.sb([128, 16], F32); rnw = kb.sb([128, 192], F32); rc_d = Dep()
        kb.dma("sp", rdec[:, :, :].rearrange("p a b -> p (a b)"), io["rdec"], wa=[rc_d])
        kb.dma("sp", rsc[:, :], io["rsc"], wa=[rc_d])
        kb.dma("sp", rnw[:, :], io["rnw"].partition_broadcast(128), wa=[rc_d])
        QrT = kb.sb([128, S], BF16); KrT = kb.sb([128, S], BF16); qrt_d = Dep()
        Kr = kb.sb([128, NCH, 128], BF16); kr_d = Dep()
        ps_o = kb.ps([128, 1024], BF16); ps_o_d = Dep()
        kb.push()
        Qr = kb.sb([128, NCH, 128], BF16); qr_d = Dep()
        Qs = kb.sb([128, NCH, 96], BF16); Ks = kb.sb([128, NCH, 96], BF16); qs_d = Dep()
        for q8 in range(8):
            kb.dma("sp", Qs[:, q8 * 8:(q8 + 1) * 8, :], tmv[:, q8 * 8:(q8 + 1) * 8, TM_RQ:TM_RQ + 96], r=[tm_d], wa=[qs_d])
            kb.dma("sp", Ks[:, q8 * 8:(q8 + 1) * 8, :], tmv[:, q8 * 8:(q8 + 1) * 8, TM_RK:TM_RK + 96], r=[tm_d], wa=[qs_d])
        rcos = kb.sb([128, NCH, 24], F32); rsin = kb.sb([128, NCH, 24], F32)
        kb.dma("sp", rcos[:, :, :].rearrange("p a b -> p (a b)"), io["rcos"], wa=[rc_d])
        kb.dma("sp", rsin[:, :, :].rearrange("p a b -> p (a b)"), io["rsin"], wa=[rc_d])
        t1 = kb.sb([128, NCH, 2, 24], F32); t2 = kb.sb([128, NCH, 2, 24], F32); t_d = Dep()
        kb.op("dve", lambda e: e.memset(Qr[:, :, :].rearrange("p a b -> p (a b)"), 0.0), w=[qr_d])
        kb.op("pool", lambda e: e.memset(Kr[:, :, :].rearrange("p a b -> p (a b)"), 0.0), w=[kr_d])
        kb.op("dve", lambda e: e.tensor_scalar(out=Ks[:, :, :].rearrange("p a b -> p (a b)"), in0=Ks[:, :, :].rearrange("p a b -> p (a b)"), scalar1=48.0 ** -0.5, scalar2=None, op0=ALU.mult), r=[qs_d], w=[qs_d])
        cos_b = rcos[:, :, :].unsqueeze(2).broadcast_to([128, NCH, 2, 24]); sin_b = rsin[:, :, :].unsqueeze(2).broadcast_to([128, NCH, 2, 24])
        for (Xs, Xr, xd) in ((Qs, Qr, qr_d), (Ks, Kr, kr_d)):
            X5 = Xs[:, :, :].rearrange("p n (h i two) -> p n h i two", h=2, two=2)
            xe = X5[:, :, :, :, 0]; xo = X5[:, :, :, :, 1]
            R5 = Xr[:, :, :].rearrange("p n (h r) -> p n h r", h=2)[:, :, :, 0:48].rearrange("p n h (i two) -> p n h i two", two=2)
            kb.op("dve", lambda e: e.tensor_tensor(out=t1[:, :, :, :], in0=xe, in1=cos_b, op=ALU.mult), r=[qs_d, rc_d], w=[t_d])
            kb.op("dve", lambda e: e.tensor_tensor(out=t2[:, :, :, :], in0=xo, in1=sin_b, op=ALU.mult), r=[qs_d, rc_d], wa=[t_d])
            kb.op("dve", lambda e: e.tensor_tensor(out=R5[:, :, :, :, 0], in0=t1[:, :, :, :], in1=t2[:, :, :, :], op=ALU.subtract), r=[t_d], w=[xd])
            kb.op("dve", lambda e: e.tensor_tensor(out=t1[:, :, :, :], in0=xe, in1=sin_b, op=ALU.mult), r=[qs_d, rc_d, xd], w=[t_d])
            kb.op("dve", lambda e: e.tensor_tensor(out=t2[:, :, :, :], in0=xo, in1=cos_b, op=ALU.mult), r=[qs_d, rc_d], wa=[t_d])
            kb.op("dve", lambda e: e.tensor_tensor(out=R5[:, :, :, :, 1], in0=t1[:, :, :, :], in1=t2[:, :, :, :], op=ALU.add), r=[t_d, xd], w=[xd])
        for (Xr, xd, XT) in ((Qr, qr_d, QrT), (Kr, kr_d, KrT)):
            for n8 in range(NCH // 8):
                for i8 in range(8):
                    n = n8 * 8 + i8
                    kb.op("pe", lambda e: e.transpose(ps_o[:, i8 * 128:(i8 + 1) * 128], Xr[:, n, :], ident_b[:, :]), r=[xd, cd],
                          w=[ps_o_d] if i8 == 0 else (), wa=() if i8 == 0 else [ps_o_d])
                kb.op("act", lambda e: e.activation(out=XT[:, n8 * 1024:(n8 + 1) * 1024], in_=ps_o[:, :], func=AF.Copy), r=[ps_o_d], wa=[qrt_d])
        kb.pop()
        V = kb.sb([128, NCH, 192], BF16); Gg = kb.sb([128, NCH, 192], BF16); vg_d = Dep(); gg_d = Dep()
        for q8 in range(8):
            kb.dma("sp", V[:, q8 * 8:(q8 + 1) * 8, :], tmv[:, q8 * 8:(q8 + 1) * 8, TM_RV:TM_RV + 192], r=[tm_d], wa=[vg_d])
            kb.dma("sp", Gg[:, q8 * 8:(q8 + 1) * 8, :], tmv[:, q8 * 8:(q8 + 1) * 8, TM_RG:TM_RG + 192], r=[tm_d], wa=[gg_d])
        ysum = kb.sb([128, NCH, 192], BF16); ys_d = [Dep() for _ in range(NCH)]
        sigmoid_inplace(Gg, gg_d, mul_by_self=True)
        Sm = [kb.sb([128, 96], F32) for _ in range(4)]; Sb = [[kb.sb([128, 96], BF16) for _ in range(2)] for _ in range(4)]
        Sm_d = [Dep() for _ in range(4)]; Sb_d = [[Dep() for _ in range(2)] for _ in range(4)]
        for c in range(4):
            kb.op("dve", lambda e: e.memset(Sm[c][:, :], 0.0), w=[Sm_d[c]])
            kb.op("dve", lambda e: e.memset(Sb[c][0][:, :], 0.0), w=[Sb_d[c][0]])
        ps_st = kb.ps([128, 512], F32); ps_st_d = Dep()
        ps_io = [kb.ps([128, 512], F32) for _ in range(2)]; ps_io_d = [Dep() for _ in range(2)]
        STd = [kb.sb([128, 128], BF16) for _ in range(2)]; STd_d = [Dep() for _ in range(2)]
        vp2 = [kb.sb([128, 2, 96], BF16) for _ in range(2)]; vp2_d = [Dep() for _ in range(2)]
        o1 = [kb.sb([128, 96], F32) for _ in range(2)]; o1_d = [Dep() for _ in range(2)]
        for d in range(2):
            order = range(NCH) if d == 0 else range(NCH - 1, -1, -1)
            for step, n in enumerate(order):
                ncol = slice(n * 128, (n + 1) * 128)
                for hl in range(2):
                    c = hl * 2 + d
                    b0 = 64 * hl
                    pb = hl
                    par = step % 2
                    rows = slice(b0, b0 + 48)
                    kb.op("pe", lambda e: e.matmul(ps_st[:, 0:128], KrT[b0:b0 + 48, ncol], QrT[b0:b0 + 48, ncol], start=True, stop=True), r=[qrt_d], w=[ps_st_d], drain=True)
                    kb.op("dve", lambda e: e.tensor_tensor(out=STd[pb][:, :], in0=ps_st[:, 0:128], in1=rdec[:, c, :], op=ALU.mult), r=[ps_st_d, rc_d], w=[STd_d[pb]])
                    if hl == 0:
                        kb.op("pool", lambda e: e.tensor_tensor(out=vp2[par][:, :, :], in0=V[:, n, :].rearrange("p (h c) -> p h c", h=2),
                                                                in1=rsc[:, d * 4:d * 4 + 9:8].unsqueeze(2).broadcast_to([128, 2, 96]), op=ALU.mult), r=[vg_d, rc_d], w=[vp2_d[par]])
                    P = ps_io[pb]
                    kb.op("pe", lambda e: e.matmul(P[:, 0:96], STd[pb][:, :], V[:, n, hl * 96:(hl + 1) * 96], start=True, stop=True), r=[STd_d[pb], vg_d], w=[ps_io_d[pb]])
                    kb.op("pe", lambda e: e.matmul(P[:, 128:224], QrT[b0:b0 + 48, ncol], Sb[c][par][b0:b0 + 48, :], start=True, stop=True), r=[qrt_d, Sb_d[c][par]], wa=[ps_io_d[pb]])
                    kb.op("pe", lambda e: e.matmul(P[0:b0 + 48, 256:352], Kr[:, n, 0:b0 + 48], vp2[par][:, hl, :], start=True, stop=True), r=[kr_d, vp2_d[par]], wa=[ps_io_d[pb]])
                    kb.op("dve", lambda e: e.tensor_copy(out=o1[pb][:, :], in_=P[:, 0:96]), r=[ps_io_d[pb]], w=[o1_d[pb]])
                    ydst = ysum[:, n, hl * 96:(hl + 1) * 96]
                    if d == 0:
                        kb.op("dve", lambda e: e.scalar_tensor_tensor(out=ydst, in0=P[:, 128:224], scalar=rsc[:, c * 4 + 1:c * 4 + 2], in1=o1[pb][:, :], op0=ALU.mult, op1=ALU.add),
                              r=[ps_io_d[pb], o1_d[pb], rc_d], w=[ys_d[n]] if hl == 0 else (), wa=() if hl == 0 else [ys_d[n]])
                    else:
                        kb.op("dve", lambda e: e.scalar_tensor_tensor(out=o1[pb][:, :], in0=P[:, 128:224], scalar=rsc[:, c * 4 + 1:c * 4 + 2], in1=o1[pb][:, :], op0=ALU.mult, op1=ALU.add),
                              r=[ps_io_d[pb], rc_d], w=[o1_d[pb]])
                        kb.op("dve", lambda e: e.tensor_tensor(out=ydst, in0=ydst, in1=o1[pb][:, :], op=ALU.add), r=[o1_d[pb], ys_d[n]], w=[ys_d[n]])
                    kb.op("dve", lambda e: e.scalar_tensor_tensor(out=Sb[c][1 - par][rows, :], in0=Sb[c][par][rows, :], scalar=rsc[rows, c * 4 + 2:c * 4 + 3], in1=P[rows, 256:352], op0=ALU.mult, op1=ALU.add),
                          r=[ps_io_d[pb], Sb_d[c][par], rc_d], w=[Sb_d[c][1 - par]])
        finalize_batched(ysum, ys_d, Gg, gg_d, rnw, 320, ps_o, ps_o_d)
        kb.pop()
    kb.barrier()


D = 1024
NE = 32
FF = 1024
ALPHA = (2.0 * 4) ** 0.25
EPS = 1e-5
SW_ALPHA = 1.702
SW_LIM = 7.0


def consts_b(C):
    c = {}
    c["ident_f"] = np.eye(128, dtype=np.float32)
    c["ustrict"] = np.triu(np.ones((128, 128), np.float32), 1)
    c["ones_f"] = np.ones((128, 128), np.float32)
    c["ec"] = np.tile((np.arange(NE, dtype=np.float32) * C)[None, :], (128, 1))
    return c


def layer_norm_tile(kb, r_t, r_d, g_t, b_t, out_t, out_d, scr, scr_d, eng2="dve", pd=()):
    st, mv, rstd = scr
    kb.op("dve", lambda e: e.bn_stats(out=st[:, 0, :], in_=r_t[:, 0:512]), r=[r_d], w=[scr_d])
    kb.op("dve", lambda e: e.bn_stats(out=st[:, 1, :], in_=r_t[:, 512:1024]), r=[r_d], wa=[scr_d])
    kb.op("dve", lambda e: e.bn_aggr(out=mv[:, :], in_=st[:, :, :].rearrange("p a b -> p (a b)")), r=[scr_d], wa=[scr_d])
    kb.op("act", lambda e: e.activation(out=rstd[:, :], in_=mv[:, 1:2], func=AF.Ln, bias=EPS), r=[scr_d], wa=[scr_d])
    kb.op("act", lambda e: e.activation(out=rstd[:, :], in_=rstd[:, :], func=AF.Exp, scale=-0.5), r=[scr_d], wa=[scr_d])
    kb.op("dve", lambda e: e.tensor_scalar(out=out_t, in0=r_t, scalar1=mv[:, 0:1], scalar2=rstd[:, 0:1],
                                           op0=ALU.subtract, op1=ALU.mult), r=[r_d, scr_d], w=[out_d])
    kb.op(eng2, lambda e: e.tensor_tensor(out=out_t, in0=out_t, in1=g_t, op=ALU.mult), r=[out_d] + list(pd), w=[out_d])
    kb.op(eng2, lambda e: e.tensor_tensor(out=out_t, in0=out_t, in1=b_t, op=ALU.add), r=[out_d] + list(pd), w=[out_d])


def build_b(nc, kb, io, NT, C, want_xT=True):
    TT = NT // 128
    NJ = C // 128
    TRASH = NE * C
    ident_f = kb.sb([128, 128], F32); ident_b = kb.sb([128, 128], BF16)
    ustrict_b = kb.sb([128, 128], BF16); ones_b = kb.sb([128, 128], BF16); ones_f = kb.sb([128, 128], F32)
    ec = kb.sb([128, NE], F32)
    cd = Dep()
    onesrow = kb.sb([1, 512], BF16)
    kb.op("dve", lambda e: e.memset(onesrow[:, :], 1.0), wa=[cd])
    kb.dma("sp", ident_f[:, :], io["ident_f"], w=[cd])
    kb.dma("sp", ones_f[:, :], io["ones_f"], wa=[cd])
    kb.dma("sp", ec[:, :], io["ec"], wa=[cd])
    kb.dma("pool", ident_b[:, :], io["ident_f"], wa=[cd])
    kb.dma("pool", ustrict_b[:, :], io["ustrict"], wa=[cd])
    kb.dma("pool", ones_b[:, :], io["ones_f"], wa=[cd])
    zrow = kb.sb([1, D], F32)
    kb.op("dve", lambda e: e.memset(zrow[:, :], 0.0), wa=[cd])
    ypad_d = Dep(); xg_d = Dep()
    kb.dma("sp", io["ypad"][TRASH:TRASH + 1, :], zrow[:, :], r=[cd], w=[ypad_d])
    kb.push()
    zt = kb.sb([128, 8 * D], BF16); zt_d = Dep()
    kb.op("pool", lambda e: e.memset(zt[:, :], 0.0), w=[zt_d])
    nrow = NE * C + 1
    r0 = 0
    while r0 < nrow:
        nr = min(1024, nrow - r0)
        if nr == 1024:
            kb.dma("sp", io["xg"][r0:r0 + 1024, :].rearrange("(p j) d -> p (j d)", p=128), zt[:, :], r=[zt_d], wa=[xg_d])
        else:
            kb.dma("sp", io["xg"][r0:r0 + nr, :], zt[0:nr, 0:D], r=[zt_d], wa=[xg_d])
        r0 += nr
    kb.pop()
    macc = kb.sb([128, NE], BF16); macc_d = Dep()
    kb.op("dve", lambda e: e.memset(macc[:, :], 0.0), w=[macc_d])
    idx = kb.sb([128, TT, 4], I32); gate = kb.sb([128, TT, 4], F32)
    idx_d = [Dep() for _ in range(TT)]

    NB = 2
    ps_mix = kb.ps([128, D], F32); ps_mix_d = Dep()
    ps_tr = kb.ps([128, D], F32); ps_tr_d = Dep()
    ps_misc = kb.ps([128, 2, 512], F32)
    ps_lg = ps_misc[:, 0, :]; ps_lg_d = Dep()
    ps_rk = ps_misc[:, 1, :]; ps_rk_d = Dep()
    ps_t = kb.ps([128, D], BF16); ps_t_d = Dep()
    kb.push()
    wout = kb.sb([128, 8, D], BF16)
    for c in range(8):
        kb.dma("pool", wout[:, c, :], io["w_out"][c * 128:(c + 1) * 128, :], wa=[cd])
    lnp = {}
    for nm in ("ln1_g", "ln1_b"):
        lnp[nm] = kb.sb([128, D], F32)
        kb.dma("sp", lnp[nm][:, :], io[nm].partition_broadcast(128), wa=[cd])
    wr = kb.sb([128, 8, NE], F32)
    kb.dma("sp", wr[:, :, :], io["w_router"].rearrange("(c p) e -> p c e", p=128), wa=[cd])
    br = kb.sb([1, NE], F32)
    kb.dma("sp", br[:, :], io["b_router"].rearrange("(o e) -> o e", o=1), wa=[cd])
    xr = [kb.sb([128, D], F32) for _ in range(NB)]; xr_d = [Dep() for _ in range(NB)]
    rt = [kb.sb([128, D], F32) for _ in range(NB)]; rt_d = [Dep() for _ in range(NB)]
    x1 = [kb.sb([128, D], F32) for _ in range(NB)]; x1_d = [Dep() for _ in range(NB)]
    scr = [(kb.sb([128, 2, 6], F32), kb.sb([128, 2], F32), kb.sb([128, 1], F32)) for _ in range(NB)]
    scr_d = [Dep() for _ in range(NB)]
    GATH = "ymg" in io
    if GATH:
        ymall = kb.sb([128, 8, NT], BF16); ymall_d = Dep()
        idxy = kb.sb([128, 8], I32)
        kb.dma("sp", idxy[:, :], io["ymidx"], w=[ymall_d])
        for c in range(8):
            ymgv = io["ymg"][c % 4].rearrange("r (h t) -> (r h) t", h=2)
            kb.dma("pool", None, None, r=[ymall_d], wa=[ymall_d],
                   fn=lambda e: e.indirect_dma_start(out=ymall[:, c, :], out_offset=None, in_=ymgv,
                                                     in_offset=bass.IndirectOffsetOnAxis(ap=idxy[:, c:c + 1], axis=0)))
        ymt = [ymall, ymall]; ymt_d = [ymall_d, ymall_d]
    else:
        ymt = [kb.sb([128, 8, 128], BF16) for _ in range(NB)]; ymt_d = [Dep() for _ in range(NB)]
    x1b = [kb.sb([128, D], BF16) for _ in range(NB)]; x1b_d = [Dep() for _ in range(NB)]
    x1T = [kb.sb([128, 8, 128], F32) for _ in range(NB)]; x1T_d = [Dep() for _ in range(NB)]
    lg = [kb.sb([128, NE], F32) for _ in range(NB)]; lg_d = [Dep() for _ in range(NB)]
    m8 = [kb.sb([128, 8], F32) for _ in range(NB)]
    msk = [kb.sb([128, NE], BF16) for _ in range(NB)]; msk_d = [Dep() for _ in range(NB)]
    sm = [kb.sb([128, 64], F32) for _ in range(NB)]
    tmp32 = [kb.sb([128, NE], F32) for _ in range(NB)]
    destf = [kb.sb([128, NE], F32) for _ in range(NB)]
    valid = [kb.sb([128, NE], F32) for _ in range(NB)]

    for i in range(TT):
        b = i % NB
        tsl = slice(i * 128, (i + 1) * 128)
        if not GATH:
            kb.dma("sp", ymt[b][:, :, :], io["ymT"][:, tsl].rearrange("(c p) t -> p c t", p=128), w=[ymt_d[b]])
        ysl = tsl if GATH else slice(0, 128)
        kb.dma("sp", xr[b][:, :], io["xres"][tsl, :], w=[xr_d[b]])
        for h in range(2):
            for c in range(8):
                kb.op("pe", lambda e: e.matmul(ps_mix[:, h * 512:(h + 1) * 512], ymt[b][:, c, ysl],
                                               wout[:, c, h * 512:(h + 1) * 512], start=(c == 0), stop=(c == 7)),
                      r=[ymt_d[b], cd], w=[ps_mix_d] if (h == 0 and c == 0) else (), wa=() if (h == 0 and c == 0) else [ps_mix_d])
        kb.op("dve", lambda e: e.scalar_tensor_tensor(out=rt[b][:, :], in0=xr[b][:, :], scalar=ALPHA, in1=ps_mix[:, :],
                                                      op0=ALU.mult, op1=ALU.add), r=[xr_d[b], ps_mix_d], w=[rt_d[b]])
        layer_norm_tile(kb, rt[b][:, :], rt_d[b], lnp["ln1_g"][:, :], lnp["ln1_b"][:, :], x1[b][:, :], x1_d[b], scr[b], scr_d[b], pd=[cd])
        kb.dma("sp", io["x1s"][tsl, :], x1[b][:, :], r=[x1_d[b]])
        kb.op("act", lambda e: e.activation(out=x1b[b][:, :], in_=x1[b][:, :], func=AF.Copy), r=[x1_d[b]], w=[x1b_d[b]])
        for c in range(8):
            kb.op("pe", lambda e: e.transpose(ps_tr[:, c * 128:(c + 1) * 128], x1[b][:, c * 128:(c + 1) * 128], ident_f[:, :]),
                  r=[x1_d[b], cd], w=[ps_tr_d] if c == 0 else (), wa=() if c == 0 else [ps_tr_d])
        kb.op("act", lambda e: e.activation(out=x1T[b][:, :, :].rearrange("p c t -> p (c t)"), in_=ps_tr[:, :], func=AF.Copy),
              r=[ps_tr_d], w=[x1T_d[b]])
        for c in range(8):
            kb.op("pe", lambda e: e.matmul(ps_lg[:, 0:NE], x1T[b][:, c, :], wr[:, c, :], start=(c == 0), stop=False),
                  r=[x1T_d[b], cd], w=[ps_lg_d] if c == 0 else (), wa=() if c == 0 else [ps_lg_d])
        kb.op("pe", lambda e: e.matmul(ps_lg[:, 0:NE], ones_f[0:1, :], br[0:1, :], start=False, stop=True), r=[cd], wa=[ps_lg_d])
        kb.op("dve", lambda e: e.tensor_copy(out=lg[b][:, :], in_=ps_lg[:, 0:NE]), r=[ps_lg_d], w=[lg_d[b]])
        L = lg_d[b]
        kb.op("dve", lambda e: e.max(out=m8[b][:, :], in_=lg[b][:, :]), r=[L], w=[L])
        kb.op("dve", lambda e: e.tensor_scalar(out=msk[b][:, :], in0=lg[b][:, :], scalar1=m8[b][:, 3:4], scalar2=None,
                                               op0=ALU.is_ge), r=[L], w=[msk_d[b]])
        kb.op("dve", lambda e: e.tensor_scalar(out=sm[b][:, 0:1], in0=m8[b][:, 0:1], scalar1=-1.0, scalar2=None, op0=ALU.mult), r=[L], w=[L])
        kb.op("act", lambda e: e.activation(out=sm[b][:, 4:8], in_=m8[b][:, 0:4], func=AF.Exp, bias=sm[b][:, 0:1], scale=1.0), r=[L], w=[L])
        kb.op("dve", lambda e: e.reduce_sum(out=sm[b][:, 1:2], in_=sm[b][:, 4:8], axis=AX.X), r=[L], w=[L])
        kb.op("dve", lambda e: e.reciprocal(out=sm[b][:, 2:3], in_=sm[b][:, 1:2]), r=[L], w=[L])
        kb.op("dve", lambda e: e.tensor_scalar(out=sm[b][:, 8:12], in0=sm[b][:, 4:8], scalar1=sm[b][:, 2:3], scalar2=None, op0=ALU.mult), r=[L], w=[L])
        kb.op("pe", lambda e: e.matmul(ps_rk[:, 0:NE], ustrict_b[:, :], msk[b][:, :], start=True, stop=False), r=[msk_d[b], cd], w=[ps_rk_d])
        kb.op("pe", lambda e: e.matmul(ps_rk[:, 0:NE], ones_b[:, :], macc[:, :], start=False, stop=True), r=[macc_d, cd], wa=[ps_rk_d])
        kb.op("dve", lambda e: e.tensor_tensor(out=macc[:, :], in0=macc[:, :], in1=msk[b][:, :], op=ALU.add), r=[msk_d[b], ps_rk_d], w=[macc_d])
        kb.op("dve", lambda e: e.tensor_scalar(out=valid[b][:, :], in0=ps_rk[:, 0:NE], scalar1=float(C), scalar2=None, op0=ALU.is_lt), r=[ps_rk_d, L], w=[L])
        kb.op("dve", lambda e: e.scalar_tensor_tensor(out=destf[b][:, :], in0=ps_rk[:, 0:NE], scalar=-float(TRASH), in1=ec[:, :],
                                                      op0=ALU.add, op1=ALU.add), r=[ps_rk_d, L, cd], w=[L])
        kb.op("dve", lambda e: e.tensor_tensor(out=destf[b][:, :], in0=destf[b][:, :], in1=valid[b][:, :], op=ALU.mult), r=[L], w=[L])
        for k in range(4):
            kb.op("dve", lambda e: e.scalar_tensor_tensor(out=tmp32[b][:, :], in0=lg[b][:, :], scalar=m8[b][:, k:k + 1], in1=destf[b][:, :],
                                                          op0=ALU.is_equal, op1=ALU.mult), r=[L], w=[L])
            kb.op("dve", lambda e: e.reduce_sum(out=sm[b][:, 16 + k:17 + k], in_=tmp32[b][:, :], axis=AX.X), r=[L], w=[L])
            kb.op("dve", lambda e: e.scalar_tensor_tensor(out=tmp32[b][:, :], in0=lg[b][:, :], scalar=m8[b][:, k:k + 1], in1=valid[b][:, :],
                                                          op0=ALU.is_equal, op1=ALU.mult), r=[L], w=[L])
            kb.op("dve", lambda e: e.reduce_sum(out=sm[b][:, 20 + k:21 + k], in_=tmp32[b][:, :], axis=AX.X), r=[L], w=[L])
        kb.op("dve", lambda e: e.tensor_scalar(out=idx[:, i, :], in0=sm[b][:, 16:20], scalar1=float(TRASH), scalar2=None, op0=ALU.add), r=[L], w=[idx_d[i]])
        kb.op("dve", lambda e: e.tensor_tensor(out=gate[:, i, :], in0=sm[b][:, 8:12], in1=sm[b][:, 20:24], op=ALU.mult), r=[L], wa=[idx_d[i]])
        for k in range(4):
            kb.dma("pool", None, None, r=[x1b_d[b], idx_d[i]], wa=[xg_d],
                   fn=lambda e: e.indirect_dma_start(out=io["xg"], out_offset=bass.IndirectOffsetOnAxis(ap=idx[:, i, k:k + 1], axis=0),
                                                     in_=x1b[b][:, :], in_offset=None))

    kb.pop()
    kb.push()
    w1s = [kb.sb([128, 8, 2 * FF], BF16) for _ in range(2)]; w1_d = [Dep() for _ in range(2)]
    w2s = [kb.sb([128, 8, D], BF16) for _ in range(2)]; w2_d = [Dep() for _ in range(2)]
    b1r = [kb.sb([1, 2 * FF], BF16) for _ in range(2)]; b2r = [kb.sb([1, D], BF16) for _ in range(2)]
    xgs = kb.sb([128, NJ, D], BF16); xgs_d = Dep()
    xgT = kb.sb([128, 8, C], BF16); xgT_d = Dep()
    actT = kb.sb([128, 8, C], BF16); actT_d = [Dep() for _ in range(8)]
    tg = kb.sb([128, C], F32); tg_d = Dep()
    ts_ = kb.sb([128, C], F32); ts_d = Dep()
    tu = kb.sb([128, C], F32); tu_d = Dep()
    yo = [kb.sb([128, D], F32) for _ in range(2)]; yo_d = [Dep() for _ in range(2)]
    ps_g = ps_tr[:, :].rearrange("p (a b) -> p a b", a=2); ps_g_d = ps_tr_d
    ps_u = ps_misc; ps_u_d = ps_lg_d
    ps_y = ps_mix; ps_y_d = ps_mix_d
    nsp = [(0, min(C, 512))] + ([(512, C)] if C > 512 else [])

    def load_w(e_):
        bb = e_ % 2
        for c2 in range(2):
            kb.dma("pool", w1s[bb][:, c2 * 4:(c2 + 1) * 4, :], io["w1"][e_, c2 * 512:(c2 + 1) * 512, :].rearrange("(c p) f -> p c f", p=128),
                   w=[w1_d[bb]] if c2 == 0 else (), wa=() if c2 == 0 else [w1_d[bb]])
        kb.dma("pool", b1r[bb][:, :], io["b1"][e_:e_ + 1, :], wa=[w1_d[bb]])
        for c2 in range(2):
            kb.dma("pool", w2s[bb][:, c2 * 4:(c2 + 1) * 4, :], io["w2"][e_, c2 * 512:(c2 + 1) * 512, :].rearrange("(c p) f -> p c f", p=128),
                   w=[w2_d[bb]] if c2 == 0 else (), wa=() if c2 == 0 else [w2_d[bb]])
        kb.dma("pool", b2r[bb][:, :], io["b2"][e_:e_ + 1, :], wa=[w2_d[bb]])

    load_w(0)
    for ex in range(NE):
        bb = ex % 2
        if ex + 1 < NE:
            load_w(ex + 1)
        kb.dma("sp", xgs[:, :, :], io["xg"][ex * C:(ex + 1) * C, :].rearrange("(j p) d -> p j d", p=128), r=[xg_d], w=[xgs_d])
        for j in range(NJ):
            for c in range(8):
                kb.op("pe", lambda e: e.transpose(ps_t[:, c * 128:(c + 1) * 128], xgs[:, j, c * 128:(c + 1) * 128], ident_b[:, :]),
                      r=[xgs_d, cd], w=[ps_t_d] if c == 0 else (), wa=() if c == 0 else [ps_t_d])
            kb.op("act", lambda e: e.activation(out=xgT[:, :, j * 128:(j + 1) * 128], in_=ps_t[:, :].rearrange("p (c t) -> p c t", c=8), func=AF.Copy),
                  r=[ps_t_d], w=[xgT_d] if j == 0 else (), wa=() if j == 0 else [xgT_d])
        for m in range(8):
            for (pst, pd, col0) in ((ps_g, ps_g_d, m * 128), (ps_u, ps_u_d, FF + m * 128)):
                first = True
                for hi, (n0, n1) in enumerate(nsp):
                    kb.op("pe", lambda e: e.matmul(pst[:, hi, 0:n1 - n0], b1r[bb][0:1, col0:col0 + 128], onesrow[0:1, 0:n1 - n0], start=True, stop=False),
                          r=[w1_d[bb], cd], w=[pd] if first else (), wa=() if first else [pd])
                    first = False
                    for c in range(8):
                        kb.op("pe", lambda e: e.matmul(pst[:, hi, 0:n1 - n0], w1s[bb][:, c, col0:col0 + 128], xgT[:, c, n0:n1], start=False, stop=(c == 7)),
                              r=[w1_d[bb], xgT_d], wa=[pd])
            for hi, (n0, n1) in enumerate(nsp):
                w_ = n1 - n0
                kb.op("dve", lambda e: e.tensor_scalar(out=tg[:, n0:n1], in0=ps_g[:, hi, 0:w_], scalar1=SW_LIM, scalar2=None, op0=ALU.min),
                      r=[ps_g_d], w=[tg_d] if hi == 0 else (), wa=() if hi == 0 else [tg_d])
                kb.op("dve", lambda e: e.tensor_scalar(out=tu[:, n0:n1], in0=ps_u[:, hi, 0:w_], scalar1=SW_LIM, scalar2=-SW_LIM, op0=ALU.min, op1=ALU.max),
                      r=[ps_u_d], w=[tu_d] if hi == 0 else (), wa=() if hi == 0 else [tu_d])
            kb.op("act", lambda e: e.activation(out=ts_[:, :], in_=tg[:, :], func=AF.Sigmoid, scale=SW_ALPHA), r=[tg_d], w=[ts_d])
            kb.op("dve", lambda e: e.tensor_tensor(out=ts_[:, :], in0=ts_[:, :], in1=tg[:, :], op=ALU.mult), r=[tg_d, ts_d], w=[ts_d])
            kb.op("dve", lambda e: e.scalar_tensor_tensor(out=actT[:, m, :], in0=tu[:, :], scalar=1.0, in1=ts_[:, :], op0=ALU.add, op1=ALU.mult),
                  r=[tu_d, ts_d], w=[actT_d[m]])
        for j in range(NJ):
            yb = j % 2
            for h in range(2):
                kb.op("pe", lambda e: e.matmul(ps_y[:, h * 512:(h + 1) * 512], ones_b[0:1, :], b2r[bb][0:1, h * 512:(h + 1) * 512], start=True, stop=False),
                      r=[w2_d[bb], cd], w=[ps_y_d] if h == 0 else (), wa=() if h == 0 else [ps_y_d])
                for c in range(8):
                    kb.op("pe", lambda e: e.matmul(ps_y[:, h * 512:(h + 1) * 512], actT[:, c, j * 128:(j + 1) * 128], w2s[bb][:, c, h * 512:(h + 1) * 512],
                                                   start=False, stop=(c == 7)), r=[w2_d[bb], actT_d[c]], wa=[ps_y_d])
            kb.op("act", lambda e: e.activation(out=yo[yb][:, :], in_=ps_y[:, :], func=AF.Copy), r=[ps_y_d], w=[yo_d[yb]])
            r0 = ex * C + j * 128
            kb.dma("sp", io["ypad"][r0:r0 + 128, :], yo[yb][:, :], r=[yo_d[yb]], wa=[ypad_d])

    kb.pop()
    kb.push()
    lnp = {}
    for nm in ("ln2_g", "ln2_b"):
        lnp[nm] = kb.sb([128, D], F32)
        kb.dma("sp", lnp[nm][:, :], io[nm].partition_broadcast(128), wa=[cd])
    xr = [kb.sb([128, D], F32) for _ in range(NB)]; xr_d = [Dep() for _ in range(NB)]
    rt = [kb.sb([128, D], F32) for _ in range(NB)]; rt_d = [Dep() for _ in range(NB)]
    x1 = [kb.sb([128, D], F32) for _ in range(NB)]; x1_d = [Dep() for _ in range(NB)]
    scr = [(kb.sb([128, 2, 6], F32), kb.sb([128, 2], F32), kb.sb([128, 1], F32)) for _ in range(NB)]
    scr_d = [Dep() for _ in range(NB)]
    yk = [kb.sb([128, 4, D], F32) for _ in range(2)]; yk_d = [Dep() for _ in range(2)]
    x2b = [kb.sb([128, D], BF16) for _ in range(2)]; x2b_d = [Dep() for _ in range(2)]
    x2T = [kb.sb([128, 8, 128], BF16) for _ in range(2)]; x2T_d = [Dep() for _ in range(2)]
    for i in range(TT):
        b = i % 2
        tsl = slice(i * 128, (i + 1) * 128)
        for k in range(4):
            kb.dma("pool", None, None, r=[ypad_d, idx_d[i]], w=[yk_d[b]] if k == 0 else (), wa=() if k == 0 else [yk_d[b]],
                   fn=lambda e: e.indirect_dma_start(out=yk[b][:, k, :], out_offset=None, in_=io["ypad"],
                                                     in_offset=bass.IndirectOffsetOnAxis(ap=idx[:, i, k:k + 1], axis=0)))
        kb.dma("sp", xr[b][:, :], io["x1s"][tsl, :], w=[xr_d[b]])
        kb.op("dve", lambda e: e.tensor_scalar(out=rt[b][:, :], in0=xr[b][:, :], scalar1=ALPHA, scalar2=None, op0=ALU.mult), r=[xr_d[b]], w=[rt_d[b]])
        for k in range(4):
            kb.op("dve", lambda e: e.scalar_tensor_tensor(out=rt[b][:, :], in0=yk[b][:, k, :], scalar=gate[:, i, k:k + 1], in1=rt[b][:, :],
                                                          op0=ALU.mult, op1=ALU.add), r=[yk_d[b], idx_d[i]], w=[rt_d[b]])
        layer_norm_tile(kb, rt[b][:, :], rt_d[b], lnp["ln2_g"][:, :], lnp["ln2_b"][:, :], x1[b][:, :], x1_d[b], scr[b], scr_d[b], pd=[cd])
        kb.dma("sp", io["x2"][tsl, :], x1[b][:, :], r=[x1_d[b]])
        if want_xT:
            kb.op("act", lambda e: e.activation(out=x2b[b][:, :], in_=x1[b][:, :], func=AF.Copy), r=[x1_d[b]], w=[x2b_d[b]])
            for c in range(8):
                kb.op("pe", lambda e: e.transpose(ps_t[:, c * 128:(c + 1) * 128], x2b[b][:, c * 128:(c + 1) * 128], ident_b[:, :]),
                      r=[x2b_d[b], cd], w=[ps_t_d] if c == 0 else (), wa=() if c == 0 else [ps_t_d])
            kb.op("act", lambda e: e.activation(out=x2T[b][:, :, :].rearrange("p c t -> p (c t)"), in_=ps_t[:, :], func=AF.Copy), r=[ps_t_d], w=[x2T_d[b]])
            if isinstance(io["x2T"], list):
                for k4 in range(4):
                    kb.dma("sp", io["x2T"][k4][:, tsl].rearrange("(c p) t -> p c t", p=128), x2T[b][:, 2 * k4:2 * k4 + 2, :], r=[x2T_d[b]])
            else:
                kb.dma("sp", io["x2T"][:, tsl].rearrange("(c p) t -> p c t", p=128), x2T[b][:, :, :], r=[x2T_d[b]])
    kb.pop()
    kb.barrier()

COL_F = 0; COL_MQK = 256; COL_MV = 640; COL_MO = 1024; COL_MG = 1408; COL_RQ = 1424; COL_RK = 1616; COL_RV = 1808; COL_RG = 2192


def prep_a(inp, l, j):
    w = inp["w_in"][l]; bs = inp["b_in"][l]
    hs = [2 * j, 2 * j + 1]
    cols = []
    for blk in range(4):
        cols += [COL_MG + blk * 4 + h for h in hs]
    cols += [COL_RQ + h * 48 + i for h in hs for i in range(48)]
    cols += [COL_RK + h * 48 + i for h in hs for i in range(48)]
    cols += [COL_MV + h * 96 + i for h in hs for i in range(96)]
    cols += [COL_MO + h * 96 + i for h in hs for i in range(96)]
    cols += [COL_RV + h * 96 + i for h in hs for i in range(96)]
    cols += [COL_RG + h * 96 + i for h in hs for i in range(96)]
    o = {}
    o["wtm"] = np.ascontiguousarray(w[:, cols]); o["btm"] = np.ascontiguousarray(bs[cols])
    fcols = [COL_F + g * 64 + i for g in hs for i in range(64)]
    o["wfT"] = np.ascontiguousarray(w[:, fcols].T); o["bf"] = np.ascontiguousarray(bs[fcols])
    cw = inp["conv_w"][l]
    for nm, base in (("q", COL_MQK), ("k", COL_MQK + 192)):
        W = np.zeros((1024, 128), np.float32); B = np.zeros((128,), np.float32); CW = np.zeros((128, 3), np.float32)
        for hl, h in enumerate(hs):
            W[:, hl * 64:hl * 64 + 48] = w[:, base + h * 48:base + (h + 1) * 48]
            B[hl * 64:hl * 64 + 48] = bs[base + h * 48:base + (h + 1) * 48]
            CW[hl * 64:hl * 64 + 48, :] = cw[:, base - COL_MQK + h * 48:base - COL_MQK + (h + 1) * 48].T
        o["w" + nm] = W; o["b" + nm] = B; o["conv" + nm] = CW
    o["mnw"] = np.ascontiguousarray(inp["ml_norm_w"][l][hs[0] * 96:(hs[1] + 1) * 96])
    o["rnw"] = np.ascontiguousarray(inp["ret_norm_w"][l][hs[0] * 96:(hs[1] + 1) * 96])
    return o


def ymix_cols(j):
    hs = [2 * j, 2 * j + 1]
    cols = [g * 64 + i for g in hs for i in range(64)]
    cols += [256 + h * 96 + i for h in hs for i in range(96)]
    cols += [640 + h * 96 + i for h in hs for i in range(96)]
    return cols


def build_p0(nc, kb, io, NT):
    TT = NT // 128
    ident_b = kb.sb([128, 128], BF16); cd = Dep()
    kb.dma("pool", ident_b[:, :], io["ident_f"], w=[cd])
    g_t = kb.sb([128, D], F32); b_t = kb.sb([128, D], F32)
    kb.dma("sp", g_t[:, :], io["emb_g"].partition_broadcast(128), wa=[cd])
    kb.dma("sp", b_t[:, :], io["emb_b"].partition_broadcast(128), wa=[cd])
    xr = [kb.sb([128, D], F32) for _ in range(2)]; xr_d = [Dep() for _ in range(2)]
    x1 = [kb.sb([128, D], F32) for _ in range(2)]; x1_d = [Dep() for _ in range(2)]
    scr = [(kb.sb([128, 2, 6], F32), kb.sb([128, 2], F32), kb.sb([128, 1], F32)) for _ in range(2)]; scr_d = [Dep() for _ in range(2)]
    x2b = [kb.sb([128, D], BF16) for _ in range(2)]; x2b_d = [Dep() for _ in range(2)]
    x2T = [kb.sb([128, 8, 128], BF16) for _ in range(2)]; x2T_d = [Dep() for _ in range(2)]
    ps_t = kb.ps([128, D], BF16); ps_t_d = Dep()
    for i in range(TT):
        b = i % 2
        tsl = slice(i * 128, (i + 1) * 128)
        kb.dma("sp", xr[b][:, :], io["x"][tsl, :], w=[xr_d[b]])
        layer_norm_tile(kb, xr[b][:, :], xr_d[b], g_t[:, :], b_t[:, :], x1[b][:, :], x1_d[b], scr[b], scr_d[b], pd=[cd])
        kb.dma("sp", io["x2"][tsl, :], x1[b][:, :], r=[x1_d[b]])
        kb.op("act", lambda e: e.activation(out=x2b[b][:, :], in_=x1[b][:, :], func=AF.Copy), r=[x1_d[b]], w=[x2b_d[b]])
        for c in range(8):
            kb.op("pe", lambda e: e.transpose(ps_t[:, c * 128:(c + 1) * 128], x2b[b][:, c * 128:(c + 1) * 128], ident_b[:, :]),
                  r=[x2b_d[b], cd], w=[ps_t_d] if c == 0 else (), wa=() if c == 0 else [ps_t_d])
        kb.op("act", lambda e: e.activation(out=x2T[b][:, :, :].rearrange("p c t -> p (c t)"), in_=ps_t[:, :], func=AF.Copy), r=[ps_t_d], w=[x2T_d[b]])
        for k4 in range(4):
            kb.dma("sp", io["x2T"][k4][:, tsl].rearrange("(c p) t -> p c t", p=128), x2T[b][:, 2 * k4:2 * k4 + 2, :], r=[x2T_d[b]])
    kb.barrier()


NT_CORE = 4096
CAP = 768
DEPTH = 4
PAIRS = [[0, 1], [2, 3], [4, 5], [6, 7]]
A_W = (("wtm", [D, 968]), ("btm", [968]), ("wfT", [128, D]), ("bf", [128]), ("wq", [D, 128]), ("wk", [D, 128]), ("bq", [128]), ("bk", [128]),
       ("convq", [128, 3]), ("convk", [128, 3]), ("mnw", [192]), ("rnw", [192]))
B_W = (("w_out", [D, D]), ("ln1_g", [D]), ("ln1_b", [D]), ("ln2_g", [D]), ("ln2_b", [D]), ("w_router", [D, NE]), ("b_router", [NE]),
       ("w1", [NE, D, 2 * FF]), ("b1", [NE, 2 * FF]), ("w2", [NE, FF, D]), ("b2", [NE, D]))


def build_layer(cA_names, cB_names, first, last):
    nc = bass.Bass("TRN2", target_bir_lowering=False)
    kb = KB(nc)
    def T(name, shape, dt, kind):
        return nc.dram_tensor(name, list(shape), dt, kind=kind).ap()
    io = {}
    if first:
        io["x"] = T("x", [NT_CORE, D], F32, "ExternalInput")
        io["emb_g"] = T("emb_g", [D], F32, "ExternalInput"); io["emb_b"] = T("emb_b", [D], F32, "ExternalInput")
        io["xres"] = T("xres", [NT_CORE, D], F32, "Internal")
    else:
        io["xres"] = T("xres_in", [NT_CORE, D], F32, "ExternalInput")
        io["x2T_in"] = T("x2T_in", [D, NT_CORE], BF16, "ExternalInput")
    for nm, shp in A_W + B_W:
        io[nm] = T(nm, shp, F32, "ExternalInput")
    for nm, shp in list(cA_names.items()) + [(k, v) for k, v in cB_names.items() if k not in cA_names]:
        io[nm] = T(nm, shp, F32, "ExternalInput")
    io["ymidx"] = T("ymidx", [128, 8], I32, "ExternalInput")
    io["out"] = T("out", [NT_CORE, D], F32, "ExternalOutput")
    if not last:
        io["x2T_out"] = T("x2T_out", [D, NT_CORE], BF16, "ExternalOutput")
    for k4 in range(4):
        io.setdefault("x2T", []).append(T("x2T%d" % k4, [256, NT_CORE], BF16, "Internal"))
        io.setdefault("xTg", []).append(T("xTg%d" % k4, [512, NT_CORE], BF16, "Internal"))
        io.setdefault("ymT", []).append(T("ymT%d" % k4, [128, S], BF16, "Internal"))
        io.setdefault("ymg", []).append(T("ymg%d" % k4, [256, S], BF16, "Internal"))
    sc = {"tm": ([S, TM_N], BF16), "x1s": ([NT_CORE, D], F32), "xg": ([NE * CAP + 1, D], BF16), "ypad": ([NE * CAP + 1, D], F32)}
    for nm, (shp, dt) in sc.items():
        io[nm] = T(nm, shp, dt, "Internal")
    kb.push()
    if first:
        build_p0(nc, kb, {"x": io["x"], "emb_g": io["emb_g"], "emb_b": io["emb_b"], "ident_f": io["ident_f"], "x2": io["xres"], "x2T": io["x2T"]}, NT_CORE)
    else:
        bt = kb.sb([128, 2, NT_CORE], BF16); bd = Dep()
        for k4 in range(4):
            kb.dma("sp", bt[:, :, :], io["x2T_in"][k4 * 256:(k4 + 1) * 256, :].rearrange("(c p) t -> p c t", p=128), w=[bd])
            kb.dma("sp", io["x2T"][k4].rearrange("(c p) t -> p c t", p=128), bt[:, :, :], r=[bd])
    kb.pop()
    kb.barrier()
    for k4 in range(4):
        kb.cc(lambda e: e.collective_compute("AllGather", ALU.bypass, replica_groups=PAIRS, ins=[io["x2T"][k4].opt()], outs=[io["xTg"][k4].opt()]))
    kb.barrier()
    ioA = {k: io[k] for k in cA_names}
    for nm, _ in A_W:
        ioA[nm] = io[nm]
    ioA.update(xTg=io["xTg"], tm=io["tm"], ymT=io["ymT"])
    kb.push()
    build_a(nc, kb, ioA, None)
    kb.pop()
    for k4 in range(4):
        kb.cc(lambda e: e.collective_compute("AllGather", ALU.bypass, replica_groups=PAIRS, ins=[io["ymT"][k4].opt()], outs=[io["ymg"][k4].opt()]))
    kb.barrier()
    ioB = {k: io[k] for k in cB_names}
    for nm, _ in B_W:
        ioB[nm] = io[nm]
    x2T_dst = None if last else [io["x2T_out"][k4 * 256:(k4 + 1) * 256, :] for k4 in range(4)]
    ioB.update(ymg=io["ymg"], ymidx=io["ymidx"], xres=io["xres"], x1s=io["x1s"], xg=io["xg"], ypad=io["ypad"], x2=io["out"], x2T=x2T_dst)
    kb.push()
    build_b(nc, kb, ioB, NT_CORE, CAP, want_xT=not last)
    kb.pop()
    kb.barrier()
    return nc


def kernel(**inp):
    from concourse.bass_utils import run_bass_kernel_spmd
    inp = {k: np.asarray(v) for k, v in inp.items()}
    x = inp["x"]
    cA = [consts_a(j)[0] for j in range(2)]
    cB = consts_b(CAP)
    cAn = {k: list(v.shape) for k, v in cA[0].items()}; cBn = {k: list(v.shape) for k, v in cB.items()}
    progs = {}
    perm = ymix_cols(0) + ymix_cols(1)
    m_idx = ((np.arange(8)[None, :] // 4) * 128 + np.arange(128)[:, None]) * 2
    prev = None
    for l in range(DEPTH):
        first, last = (l == 0), (l == DEPTH - 1)
        key = (first, last)
        if key not in progs:
            progs[key] = build_layer(cAn, cBn, first, last)
        pa = [prep_a(inp, l, j) for j in range(2)]
        shared = {nm: inp[nm][l] for nm, _ in B_W if nm != "w_out"}
        shared["w_out"] = np.ascontiguousarray(inp["w_out"][l][perm, :])
        in_maps = []
        for b in range(4):
            for j in range(2):
                c = b * 2 + j
                m = {"ymidx": (m_idx + j).astype(np.int32)}
                if first:
                    m.update(x=np.ascontiguousarray(x[b, j * NT_CORE:(j + 1) * NT_CORE]), emb_g=inp["emb_ln_g"], emb_b=inp["emb_ln_b"])
                else:
                    m.update(xres_in=prev[c]["out"], x2T_in=prev[c]["x2T_out"])
                m.update(shared); m.update(pa[j]); m.update(cB); m.update(cA[j])
                in_maps.append(m)
        prev = run_bass_kernel_spmd(progs[key], in_maps, core_ids=list(range(8))).results
    out = np.empty((4, S, D), np.float32)
    for b in range(4):
        for j in range(2):
            out[b, j * NT_CORE:(j + 1) * NT_CORE] = prev[b * 2 + j]["out"]
    return out
```
